# Optimizing a Trainium2 kernel written in Bass

```python
import math
import jax, jax.numpy as jnp
from jax import lax
import numpy as np

D_MODEL = 1024
BATCH = 8
SEQ = 4096
DEPTH = 1

N_META = 16
ATT_HEADS = 4
ATT_HEAD_DIM = 64
ATT_V_DIM = 2 * ATT_HEAD_DIM
REC_HEADS = 4
REC_DK = 128
REC_DV = 128
ATT_WIDTH = ATT_HEADS * ATT_V_DIM
REC_WIDTH = REC_HEADS * REC_DV
MIX_WIDTH = ATT_WIDTH + REC_WIDTH
COL_SIZES = (
    ATT_HEADS * 2 * ATT_HEAD_DIM,
    ATT_HEADS * 2 * ATT_HEAD_DIM,
    ATT_WIDTH,
    REC_HEADS * REC_DK,
    REC_HEADS * REC_DK,
    REC_WIDTH,
    REC_WIDTH,
)
IN_COLS = sum(COL_SIZES)
SPLIT_POINTS = tuple(int(v) for v in np.cumsum(COL_SIZES)[:-1])
Q_BLOCK = 128
REC_CHUNK = 64
ROPE_THETA = 10000.0
PEER_HEADS = 8
PEER_QUERY_DIM = 256
N_KEYS = 128
N_EXPERTS = N_KEYS * N_KEYS
PEER_TOPK = 16
PEER_BLOCK = 128
EPS = 1e-6

kernel_name = "hymba_diffattn_hgrn2_peer"


def rmsnorm(x, w):
    xf = x.astype(jnp.float32)
    y = xf * lax.rsqrt(jnp.mean(xf * xf, axis=-1, keepdims=True) + EPS)
    return (y * w.astype(jnp.float32)).astype(x.dtype)


def rope_tables(T, d):
    inv_freq = ROPE_THETA ** (-jnp.arange(0, d, 2, dtype=jnp.float32) / d)
    ang = jnp.arange(T, dtype=jnp.float32)[:, None] * inv_freq[None, :]
    ang = jnp.concatenate([ang, ang], axis=-1)
    return jnp.cos(ang), jnp.sin(ang)


def apply_rope(z, cos, sin):
    half = z.shape[-1] // 2
    rot = jnp.concatenate([-z[..., half:], z[..., :half]], axis=-1)
    return (z * cos.astype(z.dtype) + rot * sin.astype(z.dtype)).astype(z.dtype)


def lambda_init(layer_idx):
    return 0.8 - 0.6 * math.exp(-0.3 * layer_idx)


def diff_attention(aq, ak, av, lq1, lk1, lq2, lk2, subln_w, lam_init, cos, sin):
    B, T, _ = aq.shape
    q = apply_rope(aq.reshape(B, T, ATT_HEADS, 2, ATT_HEAD_DIM).transpose(3, 0, 2, 1, 4), cos, sin)
    k = apply_rope(ak.reshape(B, T, ATT_HEADS, 2, ATT_HEAD_DIM).transpose(3, 0, 2, 1, 4), cos, sin)
    v = av.reshape(B, T, ATT_HEADS, ATT_V_DIM).transpose(0, 2, 1, 3)
    q = q * (ATT_HEAD_DIM ** -0.5)
    lam = (jnp.exp(jnp.sum(lq1.astype(jnp.float32) * lk1.astype(jnp.float32)))
           - jnp.exp(jnp.sum(lq2.astype(jnp.float32) * lk2.astype(jnp.float32))) + lam_init)
    neg = jnp.finfo(jnp.float32).min

    def block(q_blk, k_blk, v_blk, q_start):
        nq, nk = q_blk.shape[-2], k_blk.shape[-2]
        mask = jnp.arange(nk)[None, :] <= (q_start + jnp.arange(nq))[:, None]
        s = jnp.einsum('pbhqd,pbhkd->pbhqk', q_blk, k_blk).astype(jnp.float32)
        p = jax.nn.softmax(jnp.where(mask, s, neg), axis=-1)
        w = p[0] - lam * p[1]
        return jnp.einsum('bhqk,bhkv->bhqv', w.astype(v_blk.dtype), v_blk)

    outs = [block(q[..., :N_META, :], k[..., :N_META, :], v[:, :, :N_META], 0)]
    for i in range((T - N_META) // Q_BLOCK):
        s0 = N_META + i * Q_BLOCK
        e0 = s0 + Q_BLOCK
        outs.append(block(q[..., s0:e0, :], k[..., :e0, :], v[:, :, :e0], s0))
    o = jnp.concatenate(outs, axis=2)
    o = rmsnorm(o, subln_w) * (1.0 - lam_init)
    return o.transpose(0, 2, 1, 3).reshape(B, T, ATT_WIDTH)


def gla_chunk(S, q, k, v, g):
    C = q.shape[2]
    cum = jnp.cumsum(g, axis=2)
    inter = jnp.einsum('bhtc,bhcv->bhtv', q * jnp.exp(cum), S)
    causal = jnp.tril(jnp.ones((C, C), dtype=bool))[:, :, None]
    diff = cum[:, :, :, None, :] - cum[:, :, None, :, :]
    decay = jnp.where(causal, jnp.exp(jnp.where(causal, diff, 0.0)), 0.0)
    A = jnp.einsum('bhtc,bhsc,bhtsc->bhts', q, k, decay)
    intra = jnp.einsum('bhts,bhsv->bhtv', A, v)
    last = cum[:, :, -1]
    S_new = (jnp.exp(last)[..., None] * S
             + jnp.einsum('bhsc,bhsv->bhcv', k * jnp.exp(last[:, :, None, :] - cum), v))
    return S_new, inter + intra


def hgrn2(rq, rf, ri, rg, lb, norm_w):
    B, T, _ = rq.shape

    def heads(z, d):
        return z.reshape(B, T, REC_HEADS, d).transpose(0, 2, 1, 3).astype(jnp.float32)

    q = jax.nn.silu(heads(rq, REC_DK))
    lbh = lb.reshape(REC_HEADS, 1, REC_DK)
    f = lbh + (1.0 - lbh) * jax.nn.sigmoid(heads(rf, REC_DK))
    k = 1.0 - f
    g = jnp.log(f)
    v = heads(ri, REC_DV)
    S0 = jnp.zeros((B, REC_HEADS, REC_DK, REC_DV), jnp.float32)
    S_meta, o_meta = gla_chunk(S0, q[:, :, :N_META], k[:, :, :N_META], v[:, :, :N_META], g[:, :, :N_META])
    n_chunks = (T - N_META) // REC_CHUNK

    def to_chunks(z):
        z = z[:, :, N_META:].reshape(B, REC_HEADS, n_chunks, REC_CHUNK, z.shape[-1])
        return z.transpose(2, 0, 1, 3, 4)

    _, o_real = lax.scan(lambda S, xs: gla_chunk(S, *xs), S_meta,
                         (to_chunks(q), to_chunks(k), to_chunks(v), to_chunks(g)))
    o_real = o_real.transpose(1, 2, 0, 3, 4).reshape(B, REC_HEADS, T - N_META, REC_DV)
    o = jnp.concatenate([o_meta, o_real], axis=2)
    o = rmsnorm(o, norm_w[:, None, :])
    o = o.transpose(0, 2, 1, 3).reshape(B, T, REC_WIDTH) * jax.nn.silu(rg.astype(jnp.float32))
    return o.astype(rq.dtype)


def token_mix(a, w_in, lb, rec_norm_w, lq1, lk1, lq2, lk2, subln_w, w_out, lam_init, cos, sin):
    proj = jnp.einsum('btd,dc->btc', a, w_in)
    aq, ak, av, rq, rf, ri, rg = jnp.split(proj, SPLIT_POINTS, axis=-1)
    att = diff_attention(aq, ak, av, lq1, lk1, lq2, lk2, subln_w, lam_init, cos, sin)
    rec = hgrn2(rq, rf, ri, rg, lb, rec_norm_w)
    mixed = jnp.concatenate([att, rec.astype(att.dtype)], axis=-1)
    return jnp.einsum('btc,cd->btd', mixed, w_out)


def peer_ffn(h, w_query, subkeys, u_table, v_table):
    B, T, D = h.shape
    n = B * T
    pad = (-n) % PEER_BLOCK
    blocks = jnp.pad(h.reshape(n, D), ((0, pad), (0, 0))).reshape(-1, PEER_BLOCK, D)

    def one_block(xb):
        q = jnp.einsum('nd,dc->nc', xb, w_query).reshape(PEER_BLOCK, PEER_HEADS, 2, PEER_QUERY_DIM // 2)
        s = jnp.einsum('nhpc,hpkc->nhpk', q, subkeys).astype(jnp.float32)
        sv, si = lax.top_k(s, PEER_TOPK)
        cand_s = (sv[:, :, 0, :, None] + sv[:, :, 1, None, :]).reshape(PEER_BLOCK, PEER_HEADS, PEER_TOPK * PEER_TOPK)
        cand_i = (si[:, :, 0, :, None] * N_KEYS + si[:, :, 1, None, :]).reshape(PEER_BLOCK, PEER_HEADS, PEER_TOPK * PEER_TOPK)
        top_s, pos = lax.top_k(cand_s, PEER_TOPK)
        experts = jnp.take_along_axis(cand_i, pos, axis=-1).reshape(PEER_BLOCK, PEER_HEADS * PEER_TOPK)
        gates = jax.nn.softmax(top_s, axis=-1).reshape(PEER_BLOCK, PEER_HEADS * PEER_TOPK)
        u = u_table[experts]
        hid = jax.nn.gelu(jnp.einsum('nkd,nd->nk', u, xb).astype(jnp.float32), approximate=False) * gates
        return jnp.einsum('nk,nkd->nd', hid.astype(xb.dtype), v_table[experts])

    y = lax.map(one_block, blocks).reshape(-1, D)[:n]
    return y.reshape(B, T, D)


def setup_inputs(seed: int = 0) -> dict:
    key = jax.random.key(seed)
    ks = jax.random.split(key, 18)
    nrm = jax.random.normal
    f32 = jnp.float32
    return {
        "x": nrm(ks[0], (BATCH, SEQ, D_MODEL), f32),
        "meta_tokens": nrm(ks[1], (N_META, D_MODEL), f32),
        "mix_norm_w": 1.0 + 0.02 * nrm(ks[2], (DEPTH, D_MODEL), f32),
        "w_in": nrm(ks[3], (DEPTH, D_MODEL, IN_COLS), f32) * D_MODEL ** -0.5,
        "rec_lb_logits": 0.5 * nrm(ks[4], (DEPTH + 1, REC_HEADS * REC_DK), f32),
        "rec_norm_w": 1.0 + 0.02 * nrm(ks[5], (DEPTH, REC_HEADS, REC_DV), f32),
        "diff_lambda_q1": 0.1 * nrm(ks[6], (DEPTH, ATT_HEAD_DIM), f32),
        "diff_lambda_k1": 0.1 * nrm(ks[7], (DEPTH, ATT_HEAD_DIM), f32),
        "diff_lambda_q2": 0.1 * nrm(ks[8], (DEPTH, ATT_HEAD_DIM), f32),
        "diff_lambda_k2": 0.1 * nrm(ks[9], (DEPTH, ATT_HEAD_DIM), f32),
        "diff_subln_w": 1.0 + 0.02 * nrm(ks[10], (DEPTH, ATT_V_DIM), f32),
        "w_out": nrm(ks[11], (DEPTH, MIX_WIDTH, D_MODEL), f32) * MIX_WIDTH ** -0.5,
        "ffn_norm_w": 1.0 + 0.02 * nrm(ks[12], (DEPTH, D_MODEL), f32),
        "peer_w_query": nrm(ks[13], (DEPTH, D_MODEL, PEER_HEADS * PEER_QUERY_DIM), f32) * D_MODEL ** -0.5,
        "peer_subkeys": nrm(ks[14], (DEPTH, PEER_HEADS, 2, N_KEYS, PEER_QUERY_DIM // 2), f32) * (PEER_QUERY_DIM // 2) ** -0.5,
        "peer_u": nrm(ks[15], (DEPTH, N_EXPERTS, D_MODEL), f32) * D_MODEL ** -0.5,
        "peer_v": nrm(ks[16], (DEPTH, N_EXPERTS, D_MODEL), f32) * PEER_HEADS ** -0.5,
        "final_norm_w": 1.0 + 0.02 * nrm(ks[17], (D_MODEL,), f32),
    }


def reference(x, meta_tokens, mix_norm_w, w_in, rec_lb_logits, rec_norm_w,
              diff_lambda_q1, diff_lambda_k1, diff_lambda_q2, diff_lambda_k2,
              diff_subln_w, w_out, ffn_norm_w, peer_w_query, peer_subkeys,
              peer_u, peer_v, final_norm_w):
    B = x.shape[0]
    T = N_META + x.shape[1]
    meta = jnp.broadcast_to(meta_tokens[None].astype(x.dtype), (B, N_META, D_MODEL))
    h = jnp.concatenate([meta, x], axis=1)
    cos, sin = rope_tables(T, ATT_HEAD_DIM)
    lb_all = jnp.cumsum(jax.nn.softmax(rec_lb_logits.astype(jnp.float32), axis=0), axis=0)
    for l in range(DEPTH):
        a = rmsnorm(h, mix_norm_w[l])
        h = h + token_mix(a, w_in[l], lb_all[l], rec_norm_w[l],
                          diff_lambda_q1[l], diff_lambda_k1[l], diff_lambda_q2[l], diff_lambda_k2[l],
                          diff_subln_w[l], w_out[l], lambda_init(l), cos, sin).astype(h.dtype)
        if l == DEPTH - 1:
            h = h[:, N_META:]
        h = h + peer_ffn(rmsnorm(h, ffn_norm_w[l]), peer_w_query[l], peer_subkeys[l],
                         peer_u[l], peer_v[l]).astype(h.dtype)
    return rmsnorm(h, final_norm_w)
```

```python
import numpy as np
from contextlib import ExitStack
import concourse.bass as bass
import concourse.mybir as mybir
from concourse.bass_utils import run_bass_kernel_spmd

F32 = mybir.dt.float32
BF16 = mybir.dt.bfloat16
U32 = mybir.dt.uint32
ALU = mybir.AluOpType
AF = mybir.ActivationFunctionType
AX = mybir.AxisListType

T = 4112
NT = 4096
NMETA = 16
EPS = 1e-6
ENG = ['pe', 'act', 'dve', 'pool', 'sp']
SEM_LIMIT = 30000


class Tok:
    __slots__ = ('sem', 'val', 'eng', 'dma')

    def __init__(self, sem, val, eng, dma):
        self.sem = sem; self.val = val; self.eng = eng; self.dma = dma


class Prog:
    def __init__(self, nc, stack):
        self.nc = nc; self.stack = stack
        self.ops = {e: [] for e in ENG}
        self.esem = {}; self.ecnt = {}
        self.waited = {e: {} for e in ENG}
        self.lastw = {}; self.readers = {}
        self.dsem = {}
        self.nsem = 0
        self.allsems = {}
        self.rr = 0
        self.bg = set()

    def newsem(self):
        self.nsem += 1
        s = self.stack.enter_context(self.nc.semaphore(f"s{self.nsem}"))
        self.allsems[id(s)] = [s, 0]
        return s

    def _tok(self, eng, dma_key):
        if dma_key is None:
            if eng not in self.esem or self.ecnt[eng] >= SEM_LIMIT:
                self.esem[eng] = self.newsem(); self.ecnt[eng] = 0
            self.ecnt[eng] += 1
            t = Tok(self.esem[eng], self.ecnt[eng], eng, False)
        else:
            if dma_key not in self.dsem or self.dsem[dma_key][1] >= SEM_LIMIT:
                self.dsem[dma_key] = [self.newsem(), 0]
            self.dsem[dma_key][1] += 16
            t = Tok(self.dsem[dma_key][0], self.dsem[dma_key][1], eng, True)
        self.allsems[id(t.sem)][1] = t.val
        return t

    def add(self, eng, fn, reads=(), writes=(), dma_key=None):
        need = {}

        def want(t, kind):
            if (not t.dma) and t.eng == eng and dma_key is None and eng == 'pe':
                return
            k = id(t.sem)
            if k not in need or need[k][1] < t.val:
                need[k] = (t.sem, t.val)
        for k in reads:
            t = self.lastw.get(k)
            if t is not None:
                want(t, 'raw')
        for k in writes:
            t = self.lastw.get(k)
            if t is not None:
                want(t, 'waw')
            for r in self.readers.get(k, {}).values():
                want(r, 'war')
        waits = []
        wd = self.waited[eng]
        for k, (sem, val) in need.items():
            if wd.get(k, 0) >= val:
                continue
            wd[k] = val
            waits.append((sem, val))
        tok = self._tok(eng, dma_key)
        inc = 16 if dma_key is not None else 1

        def emit(e):
            for s, v in waits[1:]:
                e.wait_ge(s, v)
            inst = fn(e)
            if waits:
                inst._wait_ge(waits[0][0], waits[0][1])
            inst.then_inc(tok.sem, inc)
        self.ops[eng].append(emit)
        for k in reads:
            self.readers.setdefault(k, {})[id(tok.sem)] = tok
        for k in writes:
            self.lastw[k] = tok
            self.readers[k] = {}
        return tok

    def barrier(self):
        for e in ENG:
            wd = self.waited[e]
            for k, (s, v) in self.allsems.items():
                if k in self.bg:
                    continue
                if v > 0 and wd.get(k, 0) < v:
                    wd[k] = v
                    self.ops[e].append(lambda en, s=s, v=v: en.wait_ge(s, v))
        self.lastw = {}; self.readers = {}

    def flush(self):
        ops = self.ops
        with self.nc.Block() as block:
            @block.sync
            def _(e):
                for f in ops['sp']:
                    f(e)

            @block.tensor
            def _(e):
                for f in ops['pe']:
                    f(e)

            @block.vector
            def _(e):
                for f in ops['dve']:
                    f(e)

            @block.scalar
            def _(e):
                for f in ops['act']:
                    f(e)

            @block.gpsimd
            def _(e):
                for f in ops['pool']:
                    f(e)
        self.ops = {e: [] for e in ENG}

    def mm(self, out, lhsT, rhs, start, stop, r, w):
        return self.add('pe', lambda e: e.matmul(out, lhsT, rhs, start=start, stop=stop), r, w)

    def tr(self, out, in_, ident, r, w):
        return self.add('pe', lambda e: e.transpose(out, in_, ident), r, w)

    def act(self, out, in_, func, r, w, scale=1.0, bias=0.0):
        return self.add('act', lambda e: e.activation(out=out, in_=in_, func=func, bias=bias, scale=scale), r, w)

    def tt(self, eng, out, in0, in1, op, r, w):
        return self.add(eng, lambda e: e.tensor_tensor(out=out, in0=in0, in1=in1, op=op), r, w)

    def ts(self, eng, out, in0, s1, s2, op0, op1, r, w):
        if op1 is None:
            return self.add(eng, lambda e: e.tensor_scalar(out=out, in0=in0, scalar1=s1, scalar2=None, op0=op0), r, w)
        return self.add(eng, lambda e: e.tensor_scalar(out=out, in0=in0, scalar1=s1, scalar2=s2, op0=op0, op1=op1), r, w)

    def stt(self, eng, out, in0, scalar, in1, op0, op1, r, w):
        return self.add(eng, lambda e: e.scalar_tensor_tensor(out=out, in0=in0, scalar=scalar, in1=in1, op0=op0, op1=op1), r, w)

    def copy(self, eng, out, in_, r, w):
        if eng == 'act':
            return self.add('act', lambda e: e.copy(out=out, in_=in_), r, w)
        return self.add(eng, lambda e: e.tensor_copy(out=out, in_=in_), r, w)

    def dma(self, eng, out, in_, r, w, key):
        return self.add(eng, lambda e: e.dma_start(out=out, in_=in_), r, w, dma_key=key)

    def rsqrt_mean(self, out, in_, n, r, w, tmpkey=None):
        self.act(out, in_, AF.Sqrt, r, w, scale=1.0 / n, bias=EPS)
        self.add('dve', lambda e: e.reciprocal(out=out, in_=out), w, w)


def build(debug=False, phases="0ABCDE"):
    nc = bass.Bass("TRN2", target_bir_lowering=False)

    def din(name, shape, dt=F32):
        return nc.dram_tensor(name, list(shape), dt, kind="ExternalInput").ap()

    def dscr(name, shape, dt):
        if debug:
            return nc.dram_tensor(name, list(shape), dt, kind="ExternalOutput").ap()
        return nc.dram_tensor(name, list(shape), dt).ap()

    hT0 = din("hT0", [8, 128, T])
    w_in = din("w_in", [8, 128, 4608])
    mixw = din("mixw", [128, 8])
    cosT = din("cosT", [128, T])
    sinT = din("sinT", [128, T])
    lbl_f = din("lbl_f", [128, 8])
    lbl_t = din("lbl_t", [1, 1024])
    recnw = din("recnw", [128, 4])
    sublnw = din("sublnw", [128, 1])
    lamp = din("lamp", [1, 256])
    w_out = din("w_out", [8, 128, 1024])
    ffnw = din("ffnw", [128, 8])
    finw = din("finw", [128, 8])
    wq = din("wq", [8, 128, 2048])
    skT = din("skT", [128, 2048])
    u2 = din("u2", [128, 128, 1024])
    v2 = din("v2", [128, 128, 1024])
    c_ident = din("c_ident", [128, 128])
    c_U = din("c_U", [64, 64])
    c_L = din("c_L", [64, 64])
    c_mask = din("c_mask", [128, 2048])
    c_iota = din("c_iota", [128, 128])

    outT = nc.dram_tensor("outT", [8, 128, NT], F32, kind="ExternalOutput").ap()

    qT_s = dscr("qT_s", [4, 128, T], BF16)
    kT_s = dscr("kT_s", [4, 128, T], BF16)
    v_s = dscr("v_s", [T, 512], BF16)
    rqT_s = dscr("rqT_s", [4, 128, T], F32)
    rkT_s = dscr("rkT_s", [4, 128, T], F32)
    rgT_s = dscr("rgT_s", [4, 128, T], F32)
    rk_s = dscr("rk_s", [T, 512], F32)
    rg_s = dscr("rg_s", [T, 512], F32)
    ri_s = dscr("ri_s", [T, 512], BF16)
    mixT_s = dscr("mixT_s", [8, 128, T], BF16)
    h1T_s = dscr("h1T_s", [8, 128, T], F32)
    hnT_s = dscr("hnT_s", [8, 128, T], BF16)
    u2b = nc.dram_tensor("u2b", [128, 128, 1024], BF16).ap()
    v2b = nc.dram_tensor("v2b", [128, 128, 1024], BF16).ap()
    Gs = nc.dram_tensor("Gs", [32, 128, 128 * 128], BF16).ap()

    blocks = [(i * 512, 512) for i in range(8)] + [(4096, 16)]

    with ExitStack() as top:
        P = Prog(nc, top)

        def SB(st, name, shape, dt):
            return st.enter_context(nc.sbuf_tensor(name, list(shape), dt))

        def PS(st, name, shape, dt=F32):
            return st.enter_context(nc.psum_tensor(name, list(shape), dt))

        ones_f = SB(top, "ones_f", [128, 128], F32)
        ones_b = SB(top, "ones_b", [128, 128], BF16)
        ident = SB(top, "ident", [128, 128], F32)
        P.add('dve', lambda e: e.memset(ones_f[:], 1.0), (), ['ones_f'])
        P.add('dve', lambda e: e.memset(ones_b[:], 1.0), (), ['ones_b'])
        P.dma('sp', ident[:], c_ident, (), ['ident'], 'ident')

        if 'A' in phases:
            with ExitStack() as st:
                aT = SB(st, "aT", [128, 8, 1024], BF16)
                wb = SB(st, "wb", [128, 8, 4608], BF16)
                mw = SB(st, "mw", [128, 8], F32)
                lbf = SB(st, "lbf", [128, 8], F32)
                omlf = SB(st, "omlf", [128, 4], F32)
                lbt = SB(st, "lbt", [128, 1024], F32)
                omlt = SB(st, "omlt", [128, 512], F32)
                P.dma('sp', mw[:], mixw, (), ['mw'], 'mw')
                P.dma('sp', lbf[:], lbl_f, (), ['lbf'], 'lbf')
                P.dma('sp', lbt[:], lbl_t.partition_broadcast(128), (), ['lbt'], 'lbt')
                P.tt('dve', omlf[:], lbf[:, 4:8], lbf[:, 0:4], ALU.subtract, ['lbf'], ['omlf'])
                P.act(omlf[:], omlf[:], AF.Sigmoid, ['omlf'], ['omlf'])
                P.tt('dve', omlt[:], lbt[:, 512:1024], lbt[:, 0:512], ALU.subtract, ['lbt'], ['omlt'])
                P.act(omlt[:], omlt[:], AF.Sigmoid, ['omlt'], ['omlt'])
                for cg in range(9):
                    P.dma('pool', wb[:, :, cg * 512:(cg + 1) * 512], w_in[:, :, cg * 512:(cg + 1) * 512].rearrange("k p c -> p k c"),
                          (), ['wb'], 'wb')
                bg_list = []
                if '0' in phases:
                    for (src, dst, key) in ((u2, u2b, 'p0u'), (v2, v2b, 'p0v')):
                        for c0 in range(0, 128, 8):
                            bg_list.append((dst[c0:c0 + 8], src[c0:c0 + 8], key))

                def emit_bg(n):
                    for _ in range(n):
                        if bg_list:
                            d_, s_, k_ = bg_list.pop(0)
                            t = P.dma('pool', d_, s_, (), [], k_)
                            P.bg.add(id(t.sem))
                with ExitStack() as st2:
                    cs = [SB(st2, f"cs{i}", [128, 2, 512], F32) for i in range(2)]
                    NSTG = 6
                    stg = [SB(st2, f"stg{i}", [128, 512], F32) for i in range(NSTG)]
                    stgb = [SB(st2, f"stgb{i}", [128, 512], BF16) for i in range(NSTG)]
                    tmp = [SB(st2, f"tmp{i}", [128, 512], F32) for i in range(4)]
                    pp = [PS(st2, f"pp{i}", [128, 512]) for i in range(6)]
                    cnt = {'pp': 0, 'stg': 0, 'stgb': 0, 'tmp': 0}
                    xt = SB(st2, "xt0", [128, 8, 512], F32)
                    sq = SB(st2, "sq", [128, 8, 512], F32)
                    rstd = SB(st2, "rstd", [128, 512], F32)
                    ssp = PS(st2, "ssp", [128, 512])

                    def aoff(t):
                        return ((t // 512) % 2) * 512 + (t % 512)

                    def akey(t):
                        return f'aT{(t // 512) % 2}'

                    def norm(bi):
                        t0, n = blocks[bi]
                        P.dma('sp', xt[:, :, :n], hT0[:, :, t0:t0 + n].rearrange("k p t -> p k t"), (), ['xt0'], 'xt0')
                        P.act(sq[:, :, :n], xt[:, :, :n], AF.Square, ['xt0'], ['sq'])
                        for k in range(8):
                            P.mm(ssp[:, :n], ones_f[:], sq[:, k, :n], k == 0, k == 7, ['ones_f', 'sq'], ['ssp'])
                        P.rsqrt_mean(rstd[:, :n], ssp[:, :n], 1024.0, ['ssp'], ['rstd'])
                        o = aoff(t0)
                        for k in range(8):
                            P.stt('dve', aT[:, k, o:o + n], xt[:, k, :n], mw[:, k:k + 1], rstd[:, :n],
                                  ALU.mult, ALU.mult, ['xt0', 'mw', 'rstd'], [akey(t0)])

                    def nxt(kind, n):
                        cnt[kind] += 1
                        return cnt[kind] % n

                    def proj_f(pi, col0, t0, n):
                        for k in range(8):
                            P.mm(pp[pi][:, :n], wb[:, k, col0:col0 + 128], aT[:, k, aoff(t0):aoff(t0) + n], k == 0, k == 7,
                                 ['wb', akey(t0)], [f'pp{pi}'])

                    def proj_t(pi, col0, t0, r):
                        for k in range(8):
                            P.mm(pp[pi][:r, :], aT[:, k, aoff(t0):aoff(t0) + r], wb[:, k, col0:col0 + 512], k == 0, k == 7,
                                 ['wb', akey(t0)], [f'pp{pi}'])

                    norm(0)
                    for bi, (t0, n) in enumerate(blocks):
                        c = bi % 2
                        emit_bg(4)
                        P.dma('sp', cs[c][:, 0, :n], cosT[:, t0:t0 + n], (), [f'cs{c}'], f'cs{c}')
                        P.dma('sp', cs[c][:, 1, :n], sinT[:, t0:t0 + n], (), [f'cs{c}'], f'cs{c}')
                        for gi, (base, dst) in enumerate(((0, qT_s), (1024, kT_s))):
                            for h in range(4):
                                pa = nxt('pp', 6); proj_f(pa, base + h * 128, t0, n)
                                pb = nxt('pp', 6); proj_f(pb, base + 512 + h * 128, t0, n)
                                t1 = nxt('tmp', 4)
                                P.tt('dve', tmp[t1][:, :n], pp[pa][:, :n], cs[c][:, 0, :n], ALU.mult, [f'pp{pa}', f'cs{c}'], [f'tmp{t1}'])
                                t2 = nxt('tmp', 4)
                                P.tt('dve', tmp[t2][:, :n], pp[pb][:, :n], cs[c][:, 1, :n], ALU.mult, [f'pp{pb}', f'cs{c}'], [f'tmp{t2}'])
                                sb_ = nxt('stgb', NSTG)
                                P.tt('pool', stgb[sb_][:, :n], tmp[t1][:, :n], tmp[t2][:, :n], ALU.add, [f'tmp{t1}', f'tmp{t2}'], [f'stgb{sb_}'])
                                P.dma('pool', dst[h, :, t0:t0 + n], stgb[sb_][:, :n], [f'stgb{sb_}'], [], f'stgb{sb_}')
                        if bi + 1 < len(blocks):
                            norm(bi + 1)
                        for (base, dst) in ((2560, rqT_s), (4096, rgT_s)):
                            for h in range(4):
                                pa = nxt('pp', 6); proj_f(pa, base + h * 128, t0, n)
                                s_ = nxt('stg', NSTG)
                                P.act(stg[s_][:, :n], pp[pa][:, :n], AF.Silu, [f'pp{pa}'], [f'stg{s_}'])
                                P.dma('pool', dst[h, :, t0:t0 + n], stg[s_][:, :n], [f'stg{s_}'], [], f'stg{s_}')
                        for h in range(4):
                            pa = nxt('pp', 6); proj_f(pa, 3072 + h * 128, t0, n)
                            t1 = nxt('tmp', 4)
                            P.act(tmp[t1][:, :n], pp[pa][:, :n], AF.Sigmoid, [f'pp{pa}'], [f'tmp{t1}'], scale=-1.0)
                            s_ = nxt('stg', NSTG)
                            P.ts('pool', stg[s_][:, :n], tmp[t1][:, :n], omlf[:, h:h + 1], None, ALU.mult, None, [f'tmp{t1}', 'omlf'], [f'stg{s_}'])
                            P.dma('pool', rkT_s[h, :, t0:t0 + n], stg[s_][:, :n], [f'stg{s_}'], [], f'stg{s_}')
                        for r0 in range(0, n, 128):
                            r = min(128, n - r0)
                            tt0 = t0 + r0
                            for (base, dst) in ((2048, v_s), (3584, ri_s)):
                                pa = nxt('pp', 6); proj_t(pa, base, tt0, r)
                                sb_ = nxt('stgb', NSTG)
                                P.copy('act', stgb[sb_][:r, :], pp[pa][:r, :], [f'pp{pa}'], [f'stgb{sb_}'])
                                P.dma('pool', dst[tt0:tt0 + r, :], stgb[sb_][:r, :], [f'stgb{sb_}'], [], f'stgb{sb_}')
                            pa = nxt('pp', 6); proj_t(pa, 3072, tt0, r)
                            t1 = nxt('tmp', 4)
                            P.act(tmp[t1][:r, :], pp[pa][:r, :], AF.Sigmoid, [f'pp{pa}'], [f'tmp{t1}'], scale=-1.0)
                            s_ = nxt('stg', NSTG)
                            P.tt('pool', stg[s_][:r, :], tmp[t1][:r, :], omlt[:r, :], ALU.mult, [f'tmp{t1}', 'omlt'], [f'stg{s_}'])
                            P.dma('pool', rk_s[tt0:tt0 + r, :], stg[s_][:r, :], [f'stg{s_}'], [], f'stg{s_}')
                            s2 = nxt('stg', NSTG)
                            P.act(stg[s2][:r, :], stg[s_][:r, :], AF.Ln, [f'stg{s_}'], [f'stg{s2}'], scale=-1.0, bias=1.0)
                            P.dma('pool', rg_s[tt0:tt0 + r, :], stg[s2][:r, :], [f'stg{s2}'], [], f'stg{s2}')
                    emit_bg(64)
                    P.barrier(); P.flush()

        if 'B' in phases:
            with ExitStack() as st:
                kTh = [SB(st, f"kTh{i}", [64, 2, T], BF16) for i in range(2)]
                vh = [SB(st, f"vh{i}", [128, 33, 128], BF16) for i in range(2)]
                mskf = SB(st, "mskf", [128, 2048], F32)
                msk = SB(st, "msk", [128, 4, 512], BF16)
                qb_ = [SB(st, f"qb{i}", [64, 2, 512], BF16) for i in range(2)]
                pb_ = [SB(st, f"pb{i}", [128, 512], BF16) for i in range(8)]
                lam4 = SB(st, "lam4", [128, 256], F32)
                lamt = SB(st, "lamt", [128, 64], F32)
                lam2 = SB(st, "lam2", [128, 2], F32)
                neglam = SB(st, "neglam", [128, 1], F32)
                sw = SB(st, "sw", [128, 1], F32)
                rr = [SB(st, f"rr{i}", [128, 512], F32) for i in range(2)]
                to = [SB(st, f"to{i}", [128, 512], F32) for i in range(2)]
                oo = SB(st, "oo", [128, 512], F32)
                o2 = SB(st, "o2", [128, 512], F32)
                rs = SB(st, "rs", [128, 512], F32)
                ob = [SB(st, f"ob{i}", [128, 512], BF16) for i in range(2)]
                pS = [PS(st, f"pS{i}", [128, 512]) for i in range(4)]
                pO = [PS(st, f"pO{i}", [128, 512]) for i in range(2)]
                pL = [PS(st, f"pL{i}", [128, 512]) for i in range(2)]
                pN = pS[0]
                P.dma('sp', mskf[:], c_mask, (), ['mskf'], 'mskf')
                P.copy('dve', msk[:].rearrange("p a b -> p (a b)"), mskf[:], ['mskf'], ['msk'])
                P.dma('sp', lam4[:], lamp.partition_broadcast(128), (), ['lam4'], 'lam4')
                P.dma('sp', sw[:], sublnw, (), ['sw'], 'sw')
                P.ts('dve', sw[:], sw[:], 0.8, None, ALU.mult, None, ['sw'], ['sw'])
                for j in range(2):
                    P.tt('dve', lamt[:], lam4[:, j * 128:j * 128 + 64], lam4[:, j * 128 + 64:j * 128 + 128], ALU.mult, ['lam4'], ['lamt'])
                    P.add('dve', lambda e, j=j: e.reduce_sum(out=lam2[:, j:j + 1], in_=lamt[:], axis=AX.X), ['lamt'], ['lam2'])
                P.act(lam2[:], lam2[:], AF.Exp, ['lam2'], ['lam2'])
                P.tt('dve', neglam[:], lam2[:, 1:2], lam2[:, 0:1], ALU.subtract, ['lam2'], ['neglam'])
                P.ts('dve', neglam[:], neglam[:], -0.2, None, ALU.add, None, ['neglam'], ['neglam'])
                pbi = 0; qi = 0; oi = 0; pendB = []; pendF = []

                def fin_b(h, qs, nq, osl):
                    for comp in range(2):
                        P.add('dve', lambda e, comp=comp, nq=nq: e.reciprocal(out=rr[comp][:, :nq], in_=rr[comp][:, :nq]),
                              [f'rr{comp}'], [f'rr{comp}'])
                        P.tt('dve', to[comp][:, :nq], to[comp][:, :nq], rr[comp][:, :nq], ALU.mult, [f'to{comp}', f'rr{comp}'], [f'to{comp}'])
                    P.stt('dve', oo[:, :nq], to[1][:, :nq], neglam[:, 0:1], to[0][:, :nq], ALU.mult, ALU.add,
                          ['to0', 'to1', 'neglam'], ['oo'])
                    P.tt('dve', o2[:, :nq], oo[:, :nq], oo[:, :nq], ALU.mult, ['oo'], ['o2'])
                    P.mm(pN[:, :nq], ones_f[:], o2[:, :nq], True, True, ['ones_f', 'o2'], ['pS0'])
                    P.act(rs[:, :nq], pN[:, :nq], AF.Ln, ['pS0'], ['rs'], scale=1.0 / 128.0, bias=EPS)
                    P.act(rs[:, :nq], rs[:, :nq], AF.Exp, ['rs'], ['rs'], scale=-0.5)
                    P.stt('dve', ob[osl][:, :nq], oo[:, :nq], sw[:, 0:1], rs[:, :nq], ALU.mult, ALU.mult, ['oo', 'sw', 'rs'], [f'ob{osl}'])
                    P.dma('pool', mixT_s[h, :, qs:qs + nq], ob[osl][:, :nq], [f'ob{osl}'], ['mixT_s'], f'ob{osl}')

                def emit_ol(comp, kr, kt, psl, nq, nkt, hs):
                    P.mm(pO[comp][:, :nq], vh[hs][:kr, kt, :], pb_[psl][:kr, :nq], kt == 0, kt == nkt - 1,
                         [f'vh{hs}', f'pb{psl}'], [f'pO{comp}'])
                    P.mm(pL[comp][:, :nq], ones_b[:kr, :], pb_[psl][:kr, :nq], kt == 0, kt == nkt - 1,
                         ['ones_b', f'pb{psl}'], [f'pL{comp}'])

                for h in range(4):
                    hs = h % 2
                    P.dma('sp', kTh[hs][:], kT_s[h].rearrange("(c d) t -> d c t", c=2), ['kT_s'], [f'kTh{hs}'], f'kTh{hs}')
                    P.dma('sp', vh[hs][:, 0:32, :], v_s[0:4096, h * 128:(h + 1) * 128].rearrange("(tt p) d -> p tt d", p=128),
                          ['v_s'], [f'vh{hs}'], f'vh{hs}')
                    P.dma('sp', vh[hs][0:16, 32, :], v_s[4096:4112, h * 128:(h + 1) * 128], ['v_s'], [f'vh{hs}'], f'vh{hs}')
                    for (qs, nq) in blocks:
                        qi += 1; qsl = qi % 2
                        P.dma('sp', qb_[qsl][:, :, :nq], qT_s[h, :, qs:qs + nq].rearrange("(c d) t -> d c t", c=2),
                              ['qT_s'], [f'qb{qsl}'], f'qb{qsl}')
                        nkt = (qs + nq + 127) // 128
                        for kt in range(nkt):
                            kr = min(128, T - kt * 128)
                            for comp in range(2):
                                pbi += 1; sl = pbi % 4; psl = pbi % 8
                                P.mm(pS[sl][:kr, :nq], kTh[hs][:, comp, kt * 128:kt * 128 + kr], qb_[qsl][:, comp, :nq], True, True,
                                     [f'kTh{hs}', f'qb{qsl}'], [f'pS{sl}'])
                                P.act(pb_[psl][:kr, :nq], pS[sl][:kr, :nq], AF.Exp, [f'pS{sl}'], [f'pb{psl}'], scale=0.125)
                                if kt * 128 >= qs:
                                    j = (kt * 128 - qs) // 128
                                    P.tt('dve', pb_[psl][:kr, :nq], pb_[psl][:kr, :nq], msk[:kr, j, :nq], ALU.mult,
                                         [f'pb{psl}', 'msk'], [f'pb{psl}'])
                                pendB.append((comp, kr, kt, psl, nq, nkt, hs))
                            if len(pendB) > 2:
                                emit_ol(*pendB.pop(0)); emit_ol(*pendB.pop(0))
                            if pendF and (kt == min(3, nkt - 1)):
                                fin_b(*pendF.pop(0))
                        while pendB:
                            emit_ol(*pendB.pop(0))
                        for comp in range(2):
                            P.copy('dve', rr[comp][:, :nq], pL[comp][:, :nq], [f'pL{comp}'], [f'rr{comp}'])
                            P.copy('dve', to[comp][:, :nq], pO[comp][:, :nq], [f'pO{comp}'], [f'to{comp}'])
                        oi += 1
                        pendF.append((h, qs, nq, oi % 2))
                while pendF:
                    fin_b(*pendF.pop(0))
                P.barrier(); P.flush()

        if 'C' in phases:
            with ExitStack() as st:
                U = SB(st, "U", [64, 64], F32)
                L = SB(st, "L", [64, 64], F32)
                nw = SB(st, "nw", [128, 4], F32)
                P.dma('sp', U[:], c_U, (), ['U'], 'U')
                P.dma('sp', L[:], c_L, (), ['L'], 'L')
                P.dma('sp', nw[:], recnw, (), ['nw'], 'nw')
                gtok = [SB(st, f"gtok{i}", [64, 8, 512], F32) for i in range(2)]
                ktok = [SB(st, f"ktok{i}", [64, 8, 512], F32) for i in range(2)]
                vtok = [SB(st, f"vtok{i}", [64, 8, 512], BF16) for i in range(2)]
                rqT = [SB(st, f"rqT{i}", [128, 4, 512], F32) for i in range(2)]
                rkT = [SB(st, f"rkT{i}", [128, 4, 512], F32) for i in range(2)]
                rgT = [SB(st, f"rgT{i}", [128, 4, 512], F32) for i in range(2)]
                E1 = SB(st, "E1", [128, 4, 512], F32)
                E2 = SB(st, "E2", [128, 512], F32)
                qe = SB(st, "qe", [128, 4, 512], BF16)
                ke = SB(st, "ke", [128, 4, 512], BF16)
                Ed = SB(st, "Ed", [64, 512], F32)
                kd = SB(st, "kd", [64, 8, 512], BF16)
                ATm = [SB(st, f"ATm{i}", [64, 64], BF16) for i in range(8)]
                S = [SB(st, f"S{i}", [128, 128], F32) for i in range(4)]
                Sb = [SB(st, f"Sb{i}", [128, 128], BF16) for i in range(4)]
                o2 = SB(st, "co2", [128, 512], F32)
                rs = SB(st, "crs", [128, 512], F32)
                rec = SB(st, "rec", [128, 512], F32)
                recb = [SB(st, f"recb{i}", [128, 512], BF16) for i in range(2)]
                pC = PS(st, "pC", [128, 512])
                pF = PS(st, "pF", [128, 512])
                pA = PS(st, "pA", [64, 8, 64])
                pOo = [PS(st, f"pOo{i}", [128, 512]) for i in range(4)]
                pSp = PS(st, "pSp", [128, 4, 128])
                for h in range(4):
                    P.add('dve', lambda e, h=h: e.memset(S[h][:], 0.0), (), [f'S{h}'])
                    P.add('pool', lambda e, h=h: e.memset(Sb[h][:], 0.0), (), [f'Sb{h}'])
                ri_ = 0
                for bi, (t0, n) in enumerate(blocks):
                    s = bi % 2
                    nch = (n + 63) // 64
                    cl = min(64, n)
                    P.dma('sp', gtok[s][:cl, :nch, :], rg_s[t0:t0 + n, :].rearrange("(ch p) c -> p ch c", p=cl), ['rg_s'], [f'gtok{s}'], f'gtok{s}')
                    P.dma('sp', ktok[s][:cl, :nch, :], rk_s[t0:t0 + n, :].rearrange("(ch p) c -> p ch c", p=cl), ['rk_s'], [f'ktok{s}'], f'ktok{s}')
                    P.dma('sp', vtok[s][:cl, :nch, :], ri_s[t0:t0 + n, :].rearrange("(ch p) c -> p ch c", p=cl), ['ri_s'], [f'vtok{s}'], f'vtok{s}')
                    P.dma('sp', rqT[s][:, :, :n], rqT_s[:, :, t0:t0 + n].rearrange("h p t -> p h t"), ['rqT_s'], [f'rqT{s}'], f'rqT{s}')
                    P.dma('sp', rkT[s][:, :, :n], rkT_s[:, :, t0:t0 + n].rearrange("h p t -> p h t"), ['rkT_s'], [f'rkT{s}'], f'rkT{s}')
                    P.dma('sp', rgT[s][:, :, :n], rgT_s[:, :, t0:t0 + n].rearrange("h p t -> p h t"), ['rgT_s'], [f'rgT{s}'], f'rgT{s}')
                    for h in range(4):
                        for ch in range(nch):
                            P.mm(pC[:, ch * 64:ch * 64 + cl], gtok[s][:cl, ch, h * 128:(h + 1) * 128], U[:cl, :cl], True, True,
                                 [f'gtok{s}', 'U'], ['pC'])
                        P.act(E1[:, h, :n], pC[:, :n], AF.Exp, ['pC'], [f'E1_{h}'])
                        P.act(E2[:, :n], pC[:, :n], AF.Exp, ['pC'], ['E2'], scale=-1.0)
                        P.tt('dve', qe[:, h, :n], rqT[s][:, h, :n], E1[:, h, :n], ALU.mult, [f'rqT{s}', f'E1_{h}'], [f'qe{h}'])
                        P.tt('pool', ke[:, h, :n], rkT[s][:, h, :n], E2[:, :n], ALU.mult, [f'rkT{s}', 'E2'], [f'ke{h}'])
                    for ch in range(nch):
                        P.mm(pF[:cl, :], L[:cl, :cl], gtok[s][:cl, ch, :], True, True, [f'gtok{s}', 'L'], ['pF'])
                        P.act(Ed[:cl, :], pF[:cl, :], AF.Exp, ['pF'], ['Ed'])
                        P.tt(('dve', 'pool')[ch % 2], kd[:cl, ch, :], ktok[s][:cl, ch, :], Ed[:cl, :], ALU.mult, [f'ktok{s}', 'Ed'], [f'kd{ch}'])
                    def stageA(ch):
                        c0 = ch * 64; st_ = ch % 2
                        for h in range(4):
                            P.mm(pA[:cl, st_ * 4 + h, :cl], ke[:, h, c0:c0 + cl], qe[:, h, c0:c0 + cl], True, True, [f'ke{h}', f'qe{h}'], ['pA'])

                    def stageA2(ch):
                        st_ = ch % 2
                        for h in range(4):
                            P.tt('dve', ATm[st_ * 4 + h][:cl, :cl], pA[:cl, st_ * 4 + h, :cl], U[:cl, :cl], ALU.mult, ['pA', 'U'], [f'ATm{st_}{h}'])

                    for ch in range(nch):
                        c0 = ch * 64; st_ = ch % 2
                        stageA(ch); stageA2(ch)
                        for h in range(4):
                            hc = slice(h * 128, (h + 1) * 128)
                            P.mm(pOo[h][:, c0:c0 + cl], Sb[h][:], qe[:, h, c0:c0 + cl], True, False, [f'Sb{h}', f'qe{h}'], [f'pOo{h}'])
                            P.mm(pOo[h][:, c0:c0 + cl], vtok[s][:cl, ch, hc], ATm[st_ * 4 + h][:cl, :cl], False, True, [f'vtok{s}', f'ATm{st_}{h}'], [f'pOo{h}'])
                            P.mm(pSp[:, h, :], kd[:cl, ch, hc], vtok[s][:cl, ch, hc], True, True, [f'kd{ch}', f'vtok{s}'], ['pSp'])
                        for h in range(4):
                            P.stt('dve', S[h][:], S[h][:], E1[:, h, c0 + cl - 1:c0 + cl], pSp[:, h, :], ALU.mult, ALU.add,
                                  [f'S{h}', f'E1_{h}', 'pSp'], [f'S{h}'])
                        for h in range(4):
                            P.copy('act', Sb[h][:], S[h][:], [f'S{h}'], [f'Sb{h}'])
                    for h in range(4):
                        P.act(o2[:, :n], pOo[h][:, :n], AF.Square, [f'pOo{h}'], ['co2'])
                        P.mm(pF[:, :n], ones_f[:], o2[:, :n], True, True, ['ones_f', 'co2'], ['pF'])
                        P.act(rs[:, :n], pF[:, :n], AF.Ln, ['pF'], ['crs'], scale=1.0 / 128.0, bias=EPS)
                        P.act(rs[:, :n], rs[:, :n], AF.Exp, ['crs'], ['crs'], scale=-0.5)
                        P.stt('dve', rec[:, :n], pOo[h][:, :n], nw[:, h:h + 1], rs[:, :n], ALU.mult, ALU.mult, [f'pOo{h}', 'nw', 'crs'], ['rec'])
                        ri_ += 1; rsl = ri_ % 2
                        P.tt('pool', recb[rsl][:, :n], rec[:, :n], rgT[s][:, h, :n], ALU.mult, ['rec', f'rgT{s}'], [f'recb{rsl}'])
                        P.dma('pool', mixT_s[4 + h, :, t0:t0 + n], recb[rsl][:, :n], [f'recb{rsl}'], ['mixT_s'], f'recb{rsl}')
                P.barrier(); P.flush()

        if 'D' in phases:
            with ExitStack() as st:
                wob = SB(st, "wob", [128, 8, 1024], BF16)
                fw = SB(st, "fw", [128, 8], F32)
                mixb = [SB(st, f"mixb{i}", [128, 8, 512], BF16) for i in range(2)]
                xt = [SB(st, f"dxt{i}", [128, 8, 512], F32) for i in range(2)]
                h1 = [SB(st, f"h1_{i}", [128, 8, 512], F32) for i in range(2)]
                sq = SB(st, "dsq", [128, 8, 512], F32)
                rstd = SB(st, "drstd", [128, 512], F32)
                hn = [SB(st, f"hn{i}", [128, 8, 512], BF16) for i in range(2)]
                pp = [PS(st, f"dpp{i}", [128, 512]) for i in range(4)]
                ssp = PS(st, "dssp", [128, 512])
                P.dma('sp', fw[:], ffnw, (), ['fw'], 'fw')
                for cg in range(2):
                    P.dma('pool', wob[:, :, cg * 512:(cg + 1) * 512], w_out[:, :, cg * 512:(cg + 1) * 512].rearrange("k p c -> p k c"),
                          (), ['wob'], 'wob')
                pi = [0]

                def d_proj(bi):
                    t0, n = blocks[bi]; s = bi % 2
                    P.dma('sp', mixb[s][:, :, :n], mixT_s[:, :, t0:t0 + n].rearrange("k p t -> p k t"), ['mixT_s'], [f'mixb{s}'], f'mixb{s}')
                    P.dma('sp', xt[s][:, :, :n], hT0[:, :, t0:t0 + n].rearrange("k p t -> p k t"), (), [f'dxt{s}'], f'dxt{s}')
                    for m in range(8):
                        pi[0] += 1; ps_ = pi[0] % 4
                        for k in range(8):
                            P.mm(pp[ps_][:, :n], wob[:, k, m * 128:(m + 1) * 128], mixb[s][:, k, :n], k == 0, k == 7,
                                 ['wob', f'mixb{s}'], [f'dpp{ps_}'])
                        P.tt('dve', h1[s][:, m, :n], pp[ps_][:, :n], xt[s][:, m, :n], ALU.add, [f'dpp{ps_}', f'dxt{s}'], [f'h1_{s}'])
                    P.dma('pool', h1T_s[:, :, t0:t0 + n].rearrange("k p t -> p k t"), h1[s][:, :, :n], [f'h1_{s}'], ['h1T_s'], f'h1_{s}')

                def d_post(bi):
                    t0, n = blocks[bi]; s = bi % 2
                    P.act(sq[:, :, :n], h1[s][:, :, :n], AF.Square, [f'h1_{s}'], ['dsq'])
                    for k in range(8):
                        P.mm(ssp[:, :n], ones_f[:], sq[:, k, :n], k == 0, k == 7, ['ones_f', 'dsq'], ['dssp'])
                    P.rsqrt_mean(rstd[:, :n], ssp[:, :n], 1024.0, ['dssp'], ['drstd'])
                    for k in range(8):
                        P.stt('dve', hn[s][:, k, :n], h1[s][:, k, :n], fw[:, k:k + 1], rstd[:, :n], ALU.mult, ALU.mult,
                              [f'h1_{s}', 'fw', 'drstd'], [f'hn{s}'])
                    P.dma('pool', hnT_s[:, :, t0:t0 + n].rearrange("k p t -> p k t"), hn[s][:, :, :n], [f'hn{s}'], ['hnT_s'], f'hn{s}')

                d_proj(0)
                for bi in range(len(blocks)):
                    if bi + 1 < len(blocks):
                        d_proj(bi + 1)
                    d_post(bi)
                P.barrier(); P.flush()

        if any(c in phases for c in 'Eab'):
            with ExitStack() as st:
                wqb = SB(st, "wqb", [128, 8, 2048], BF16)
                skb = SB(st, "skb", [128, 16, 128], BF16)
                iota = SB(st, "iota", [128, 128], F32)
                P.dma('sp', iota[:], c_iota, (), ['iota'], 'iota')
                for cg in range(4):
                    P.dma('pool', wqb[:, :, cg * 512:(cg + 1) * 512], wq[:, :, cg * 512:(cg + 1) * 512].rearrange("k p c -> p k c"),
                          (), ['wqb'], 'wqb')
                P.dma('pool', skb[:].rearrange("p a b -> p (a b)"), skT, (), ['skb'], 'skb')
                hnt4 = SB(st, "hnt4", [128, 8, 512], BF16)
                qpT4 = SB(st, "qpT4", [128, 16, 512], BF16)
                ssbs = [SB(st, f"ssb{i}", [128, 16, 128], F32) for i in range(2)]
                wk = SB(st, "wk", [128, 128], F32)
                sv = SB(st, "sv", [128, 16, 16], F32)
                si = SB(st, "si", [128, 16, 16], U32)
                sif = SB(st, "sif", [128, 16, 16], F32)
                cand = SB(st, "cand", [128, 8, 256], F32)
                wk2 = SB(st, "wk2", [128, 256], F32)
                tv = SB(st, "tv", [128, 8, 16], F32)
                pos = SB(st, "pos", [128, 8, 16], U32)
                au = SB(st, "au", [128, 8, 16], U32)
                bu = SB(st, "bu", [128, 8, 16], U32)
                af = SB(st, "af", [128, 8, 16], F32)
                bf = SB(st, "bf", [128, 8, 16], F32)
                ex = SB(st, "ex", [128, 8, 16], F32)
                zz = SB(st, "zz", [128, 8], F32)
                gate = SB(st, "gate", [128, 8, 16], F32)
                eq = SB(st, "eq", [128, 8, 16, 16], F32)
                eq2 = SB(st, "eq2", [128, 8, 16, 16], F32)
                idi = SB(st, "idi", [128, 8, 16], F32)
                idj = SB(st, "idj", [128, 8, 16], F32)
                tTs = [SB(st, f"tT{i}", [128, 3, 128], F32) for i in range(2)]
                OJ = [SB(st, f"OJ{i}", [128, 32, 128], BF16) for i in range(2)]
                OI = [SB(st, f"OI{i}", [128, 32, 128], BF16) for i in range(2)]
                iotab = SB(st, "iotab", [128, 128], BF16)
                iota3b = SB(st, "iota3b", [128, 32, 128], BF16)
                P.copy('dve', iotab[:], iota[:], ['iota'], ['iotab'])
                P.copy('dve', iota3b[:], iotab[:].unsqueeze(1).to_broadcast([128, 32, 128]), ['iotab'], ['iota3b'])
                Gst = [SB(st, f"Gst{i}", [128, 128, 128], BF16) for i in range(1)]
                pq = [PS(st, f"pq{i}", [128, 512]) for i in range(2)]
                psc = [PS(st, f"psc{i}", [128, 512]) for i in range(2)]
                pT = PS(st, "pT", [128, 3, 128])
                pG = [PS(st, f"pG{i}", [128, 128, 4]) for i in range(3)]
                gi = [0]
                do_a = ('E' in phases or 'a' in phases)

                def s1big(q4):
                    p0 = NMETA + q4 * 512
                    P.dma('sp', hnt4[:], hnT_s[:, :, p0:p0 + 512].rearrange("k p t -> p k t"), ['hnT_s'], ['hnt4'], 'hnt4')
                    for ch in range(16):
                        b_ = ch % 2
                        for k in range(8):
                            P.mm(pq[b_][:], wqb[:, k, ch * 128:(ch + 1) * 128], hnt4[:, k, :], k == 0, k == 7, ['wqb', 'hnt4'], [f'pq{b_}'])
                        P.copy('act', qpT4[:, ch, :], pq[b_][:], [f'pq{b_}'], ['qpT4'])

                def s1(ti):
                    s = ti % 2
                    off = (ti % 4) * 128
                    for g4 in range(4):
                        b_ = g4 % 2
                        for c4 in range(4):
                            ch = g4 * 4 + c4
                            P.mm(psc[b_][:, c4 * 128:(c4 + 1) * 128], qpT4[:, ch, off:off + 128], skb[:, ch, :], True, True, ['qpT4', 'skb'], [f'psc{b_}'])
                        P.copy('act', ssbs[s][:, g4 * 4:(g4 + 1) * 4, :].rearrange("p a b -> p (a b)"), psc[b_][:], [f'psc{b_}'], [f'ssb{s}'])

                def s2_parts(ti):
                    s = ti % 2
                    ssb = ssbs[s]; sk = f'ssb{s}'
                    sv4 = sv[:].rearrange("p (h two) k -> p h two k", two=2)
                    sif4 = sif[:].rearrange("p (h two) k -> p h two k", two=2)

                    def lvl1(g0, g1):
                        for g in range(g0, g1):
                            P.add('dve', lambda e, g=g: e.max(out=sv[:, g, 0:8], in_=ssb[:, g, :]), [sk], ['sv'])
                            P.add('dve', lambda e, g=g: e.max_index(out=si[:, g, 0:8], in_max=sv[:, g, 0:8], in_values=ssb[:, g, :]), [sk, 'sv'], ['si'])
                            P.add('dve', lambda e, g=g: e.match_replace(out=wk[:], in_to_replace=sv[:, g, 0:8], in_values=ssb[:, g, :], imm_value=-1e30),
                                  [sk, 'sv'], ['wk'])
                            P.add('dve', lambda e, g=g: e.max(out=sv[:, g, 8:16], in_=wk[:]), ['wk'], ['sv'])
                            P.add('dve', lambda e, g=g: e.max_index(out=si[:, g, 8:16], in_max=sv[:, g, 8:16], in_values=wk[:]), ['wk', 'sv'], ['si'])

                    def pre():
                        lvl1(0, 4)

                    def part0():
                        lvl1(4, 8)

                    def part1():
                        lvl1(8, 16)
                        P.copy('pool', sif[:], si[:], ['si'], ['sif'])
                        P.tt('pool', cand[:].rearrange("p h (a b) -> p h a b", a=16),
                             sv4[:, :, 0, :].unsqueeze(3).to_broadcast([128, 8, 16, 16]),
                             sv4[:, :, 1, :].unsqueeze(2).to_broadcast([128, 8, 16, 16]), ALU.add, ['sv'], ['cand'])

                    def part2():
                        for h in range(8):
                            P.add('dve', lambda e, h=h: e.max(out=tv[:, h, 0:8], in_=cand[:, h, :]), ['cand'], ['tv'])
                            P.add('dve', lambda e, h=h: e.max_index(out=pos[:, h, 0:8], in_max=tv[:, h, 0:8], in_values=cand[:, h, :]), ['cand', 'tv'], ['pos'])
                            P.add('dve', lambda e, h=h: e.match_replace(out=wk2[:], in_to_replace=tv[:, h, 0:8], in_values=cand[:, h, :], imm_value=-1e30),
                                  ['cand', 'tv'], ['wk2'])
                            P.add('dve', lambda e, h=h: e.max(out=tv[:, h, 8:16], in_=wk2[:]), ['wk2'], ['tv'])
                            P.add('dve', lambda e, h=h: e.max_index(out=pos[:, h, 8:16], in_max=tv[:, h, 8:16], in_values=wk2[:]), ['wk2', 'tv'], ['pos'])

                    def part3():
                        P.tt('pool', ex[:], tv[:], tv[:, :, 0:1].to_broadcast([128, 8, 16]), ALU.subtract, ['tv'], ['ex'])
                        P.act(ex[:], ex[:], AF.Exp, ['ex'], ['ex'])
                        P.add('dve', lambda e: e.reduce_sum(out=zz[:], in_=ex[:], axis=AX.X), ['ex'], ['zz'])
                        P.add('dve', lambda e: e.reciprocal(out=zz[:], in_=zz[:]), ['zz'], ['zz'])
                        P.tt('pool', gate[:], ex[:], zz[:].unsqueeze(2).to_broadcast([128, 8, 16]), ALU.mult, ['ex', 'zz'], ['gate'])
                        P.add('dve', lambda e: e.tensor_single_scalar(out=au[:], in_=pos[:], scalar=4, op=ALU.logical_shift_right), ['pos'], ['au'])
                        P.add('dve', lambda e: e.tensor_single_scalar(out=bu[:], in_=pos[:], scalar=15, op=ALU.bitwise_and), ['pos'], ['bu'])
                        P.copy('pool', af[:], au[:], ['au'], ['af'])
                        P.copy('pool', bf[:], bu[:], ['bu'], ['bf'])
                        io16 = iota[:, 0:16].unsqueeze(1).unsqueeze(1).to_broadcast([128, 8, 16, 16])
                        for (src, half, dst, nm, eqt, ek) in ((af, 0, idi, 'idi', eq, 'eq'), (bf, 1, idj, 'idj', eq2, 'eq2')):
                            P.tt('dve', eqt[:], src[:].unsqueeze(3).to_broadcast([128, 8, 16, 16]), io16, ALU.is_equal, [('af', 'bf')[half], 'iota'], [ek])
                            P.tt('dve', eqt[:], eqt[:], sif4[:, :, half, :].unsqueeze(2).to_broadcast([128, 8, 16, 16]), ALU.mult, [ek, 'sif'], [ek])
                        for (dst, nm, eqt, ek) in ((idi, 'idi', eq, 'eq'), (idj, 'idj', eq2, 'eq2')):
                            P.add('dve', lambda e, dst=dst, eqt=eqt: e.reduce_sum(out=dst[:], in_=eqt[:], axis=AX.X), [ek], [nm])
                        for j, (src, nm) in enumerate(((idi, 'idi'), (idj, 'idj'), (gate, 'gate'))):
                            P.tr(pT[:, j, :], src[:].rearrange("p h k -> p (h k)"), ident[:], [nm, 'ident'], ['pT'])
                        P.copy('act', tTs[s][:], pT[:], ['pT'], [f'tT{s}'])

                    return [pre, part0, part1, part2, part3]

                def s3(ti, parts):
                    s = ti % 2
                    tT = tTs[s]; tk = f'tT{s}'
                    gs = 0

                    def expand(pc):
                        half = pc % 2; n0 = pc * 32
                        P.act(OJ[half][:], tT[:, 1, n0:n0 + 32].unsqueeze(2).to_broadcast([128, 32, 128]), AF.Copy, [tk], [f'OJ{half}'])
                        P.act(OI[half][:], tT[:, 0, n0:n0 + 32].unsqueeze(2).to_broadcast([128, 32, 128]), AF.Copy, [tk], [f'OI{half}'])

                    def onehot(pc):
                        half = pc % 2; n0 = pc * 32
                        P.tt('dve', OJ[half][:], OJ[half][:], iota3b[:], ALU.is_equal, [f'OJ{half}', 'iota3b'], [f'OJ{half}'])
                        P.tt('dve', OI[half][:], OI[half][:], iota3b[:], ALU.is_equal, [f'OI{half}', 'iota3b'], [f'OI{half}'])
                        P.tt('pool', OI[half][:], OI[half][:], tT[:, 2, n0:n0 + 32].unsqueeze(2).to_broadcast([128, 32, 128]), ALU.mult,
                             [f'OI{half}', tk], [f'OI{half}'])

                    def gmm(pc):
                        half = pc % 2; n0 = pc * 32
                        for n4 in range(0, 32, 4):
                            gi[0] += 1; gb = gi[0] % 3
                            for q in range(4):
                                nn = n4 + q
                                P.mm(pG[gb][:, :, q], OI[half][:, nn, :], OJ[half][:, nn, :], True, True, [f'OI{half}', f'OJ{half}'], [f'pG{gb}'])
                            dstap = Gst[gs][:, :, n0 + n4:n0 + n4 + 4]
                            P.copy('act', dstap, pG[gb][:], [f'pG{gb}'], [f'Gst{gs}'])

                    if parts:
                        parts[0]()
                    expand(0); onehot(0)
                    for pc in range(4):
                        if pc + 1 < 4:
                            expand(pc + 1); onehot(pc + 1)
                        gmm(pc)
                        if parts:
                            parts[pc + 1]()
                    P.dma('pool', Gs[ti], Gst[gs][:].rearrange("i j n -> i (j n)"), [f'Gst{gs}'], ['Gs'], f'Gst{gs}')

                if do_a:
                    s1big(0); s1(0)
                    for pf in s2_parts(0):
                        pf()
                    for ti in range(32):
                        if ti + 1 < 32:
                            if (ti + 1) % 4 == 0:
                                s1big((ti + 1) // 4)
                            s1(ti + 1)
                            nparts = s2_parts(ti + 1)
                        else:
                            nparts = None
                        s3(ti, nparts)
                P.bg = set()
                P.barrier(); P.flush()

            with ExitStack() as st:
                NSUB = 4; NB = 3
                fnw = SB(st, "fnw", [128, 8], F32)
                P.dma('sp', fnw[:], finw, (), ['fnw'], 'fnw')
                hnt = [SB(st, f"bhnt{i}", [128, 8, 256], BF16) for i in range(NSUB)]
                yacc = [SB(st, f"yacc{i}", [128, 8, 256], F32) for i in range(NSUB)]
                ub = [SB(st, f"ub{i}", [128, 4, 1024], BF16) for i in range(NB)]
                vb = [SB(st, f"vb{i}", [128, 4, 1024], BF16) for i in range(NB)]
                Gg = [SB(st, f"Gg{i}", [128, 8, 4, 128], BF16) for i in range(NB)]
                ge = [SB(st, f"ge{i}", [128, 256], F32) for i in range(2)]
                Hm = [SB(st, f"Hm{i}", [128, NSUB, 4, 256], BF16) for i in range(2)]
                zsq = SB(st, "zsq", [128, 8, 256], F32)
                rstd = SB(st, "erstd", [128, 256], F32)
                ot = SB(st, "ot", [128, 8, 256], F32)
                pY = [PS(st, f"pY{i}", [128, 2, 256]) for i in range(4)]
                pH = [PS(st, f"pH{i}", [128, 512]) for i in range(2)]
                pN = PS(st, "epN", [128, 512])
                li = 0; ci = 0; yi = 0
                for ps_ in range(4 if ('E' in phases or 'b' in phases) else 0):
                    tok0 = ps_ * 1024
                    for sub in range(NSUB):
                        p0 = NMETA + tok0 + sub * 256
                        P.dma('sp', hnt[sub][:], hnT_s[:, :, p0:p0 + 256].rearrange("k p t -> p k t"), ['hnT_s'], [f'bhnt{sub}'], f'bhnt{sub}')
                        P.dma('sp', yacc[sub][:], h1T_s[:, :, p0:p0 + 256].rearrange("k p t -> p k t"), ['h1T_s'], [f'yacc{sub}'], f'yacc{sub}')
                    for g in range(32):
                        c0 = 4 * g
                        li += 1; bs = li % NB; hset = li % 2
                        P.dma('sp', ub[bs][:], u2b[c0:c0 + 4].rearrange("c p f -> p c f"), (), [f'ub{bs}'], f'ub{bs}')
                        P.dma('act', vb[bs][:], v2b[c0:c0 + 4].rearrange("c p f -> p c f"), (), [f'vb{bs}'], f'vb{bs}')
                        P.dma('pool', Gg[bs][:].rearrange("i t c n -> i t (c n)"),
                              Gs[ps_ * 8:(ps_ + 1) * 8, :, c0 * 128:(c0 + 4) * 128].rearrange("t i f -> i t f"),
                              ['Gs'], [f'Gg{bs}'], f'Gg{bs}')
                        for sub in range(NSUB):
                            for cc in range(4):
                                ci += 1; hs_ = ci % 2
                                for k in range(8):
                                    P.mm(pH[hs_][:, :256], ub[bs][:, cc, k * 128:(k + 1) * 128], hnt[sub][:, k, :], k == 0, k == 7,
                                         [f'ub{bs}', f'bhnt{sub}'], [f'pH{hs_}'])
                                P.act(ge[hs_][:], pH[hs_][:, :256], AF.Gelu, [f'pH{hs_}'], [f'ge{hs_}'])
                                P.tt('dve', Hm[hset][:, sub, cc, :].rearrange("p (t n) -> p t n", t=2),
                                     ge[hs_][:].rearrange("p (t n) -> p t n", t=2), Gg[bs][:, 2 * sub:2 * sub + 2, cc, :], ALU.mult,
                                     [f'ge{hs_}', f'Gg{bs}'], [f'Hm{hset}_{sub}'])
                        for mh in range(2):
                            for sub in range(NSUB):
                                yi += 1; ys = yi % 2
                                for m in range(4):
                                    pyt = pY[ys * 2 + m // 2]
                                    for cc in range(4):
                                        P.mm(pyt[:, m % 2, :], vb[bs][:, cc, (mh * 4 + m) * 128:(mh * 4 + m + 1) * 128], Hm[hset][:, sub, cc, :],
                                             cc == 0, cc == 3, [f'vb{bs}', f'Hm{hset}_{sub}'], [f'pY{ys * 2 + m // 2}'])
                                for m2 in range(2):
                                    ya = yacc[sub][:, mh * 4 + m2 * 2:mh * 4 + m2 * 2 + 2, :]
                                    P.tt('dve', ya, pY[ys * 2 + m2][:], ya, ALU.add, [f'pY{ys * 2 + m2}', f'yacc{sub}'], [f'yacc{sub}'])
                    for sub in range(NSUB):
                        P.act(zsq[:], yacc[sub][:], AF.Square, [f'yacc{sub}'], ['zsq'])
                        for k in range(8):
                            P.mm(pN[:, :256], ones_f[:], zsq[:, k, :], k == 0, k == 7, ['ones_f', 'zsq'], ['epN'])
                        P.rsqrt_mean(rstd[:], pN[:, :256], 1024.0, ['epN'], ['erstd'])
                        for k in range(8):
                            P.stt('dve', ot[:, k, :], yacc[sub][:, k, :], fnw[:, k:k + 1], rstd[:], ALU.mult, ALU.mult, [f'yacc{sub}', 'fnw', 'erstd'], ['ot'])
                        t_o = tok0 + sub * 256
                        P.dma('pool', outT[:, :, t_o:t_o + 256].rearrange("k p t -> p k t"), ot[:], ['ot'], ['outT'], 'ot')
                P.barrier(); P.flush()
        P.barrier(); P.flush()
    return nc


def _consts():
    ident = np.eye(128, dtype=np.float32)
    s = np.arange(64)
    U = (s[:, None] <= s[None, :]).astype(np.float32)
    L = (s[:, None] > s[None, :]).astype(np.float32)
    k = np.arange(128)[:, None]; q = np.arange(512)[None, :]
    mask = np.concatenate([((j * 128 + k) <= q).astype(np.float32) for j in range(4)], axis=1)
    iota = np.broadcast_to(np.arange(128, dtype=np.float32)[None, :], (128, 128)).copy()
    return ident, U, L, mask, iota


def _rope_tables():
    inv_freq = (np.float32(10000.0) ** (-(np.arange(0, 64, 2, dtype=np.float32)) / np.float32(64))).astype(np.float32)
    ang = (np.arange(T, dtype=np.float32)[:, None] * inv_freq[None, :]).astype(np.float32)
    ang = np.concatenate([ang, ang], axis=-1)
    cos = np.cos(ang).astype(np.float32).T
    sin = np.sin(ang).astype(np.float32).T
    sgn = np.concatenate([-np.ones(32, np.float32), np.ones(32, np.float32)])[:, None]
    cosT = np.concatenate([cos, cos], axis=0)
    sinT = np.concatenate([sin * sgn, sin * sgn], axis=0)
    return np.ascontiguousarray(cosT), np.ascontiguousarray(sinT)


def make_in_maps(x, meta_tokens, mix_norm_w, w_in, rec_lb_logits, rec_norm_w, diff_lambda_q1, diff_lambda_k1,
                 diff_lambda_q2, diff_lambda_k2, diff_subln_w, w_out, ffn_norm_w, peer_w_query, peer_subkeys,
                 peer_u, peer_v, final_norm_w, cores=range(8)):
    f = lambda a: np.ascontiguousarray(np.asarray(a, dtype=np.float32))
    x = f(x); meta = f(meta_tokens)
    w = f(w_in)[0]
    idx = np.arange(512).reshape(4, 2, 64)
    pidx = np.roll(idx, -32, axis=2).reshape(-1)
    wq_, wk_ = w[:, 0:512], w[:, 512:1024]
    w_ext = np.concatenate([wq_, wq_[:, pidx], wk_, wk_[:, pidx], w[:, 1024:]], axis=1)
    w_ext = np.ascontiguousarray(w_ext.reshape(8, 128, 4608))
    vec8 = lambda v: np.ascontiguousarray(f(v).reshape(8, 128).T)
    lbl = f(rec_lb_logits)
    lbl_f = np.ascontiguousarray(lbl.reshape(2, 4, 128).transpose(2, 0, 1).reshape(128, 8))
    lbl_t = np.ascontiguousarray(lbl.reshape(1, 1024))
    recnw = np.ascontiguousarray(f(rec_norm_w)[0].T)
    sublnw = np.ascontiguousarray(f(diff_subln_w)[0].reshape(128, 1))
    lamp = np.ascontiguousarray(np.concatenate([f(diff_lambda_q1)[0], f(diff_lambda_k1)[0], f(diff_lambda_q2)[0],
                                                f(diff_lambda_k2)[0]]).reshape(1, 256))
    wo = np.ascontiguousarray(f(w_out)[0].reshape(8, 128, 1024))
    wq2 = np.ascontiguousarray(f(peer_w_query)[0].reshape(8, 128, 2048))
    sk = f(peer_subkeys)[0]
    skT = np.ascontiguousarray(sk.transpose(3, 0, 1, 2).reshape(128, 2048))
    u = f(peer_u)[0]; v = f(peer_v)[0]
    u2 = np.ascontiguousarray(u.reshape(128, 128, 8, 128).transpose(1, 3, 2, 0).reshape(128, 128, 1024))
    v2 = np.ascontiguousarray(v.reshape(128, 128, 1024).transpose(1, 0, 2))
    ident, U, L, mask, iota = _consts()
    cosT, sinT = _rope_tables()
    shared = dict(w_in=w_ext, mixw=vec8(mix_norm_w[0]), cosT=cosT, sinT=sinT, lbl_f=lbl_f, lbl_t=lbl_t, recnw=recnw,
                  sublnw=sublnw, lamp=lamp, w_out=wo, ffnw=vec8(ffn_norm_w[0]), finw=vec8(final_norm_w), wq=wq2, skT=skT,
                  u2=u2, v2=v2, c_ident=ident, c_U=U, c_L=L, c_mask=mask, c_iota=iota)
    maps = []
    for b in cores:
        h0 = np.concatenate([meta, x[b]], axis=0)
        hT0 = np.ascontiguousarray(h0.T.reshape(8, 128, T))
        m = dict(shared); m["hT0"] = hT0
        maps.append(m)
    return maps


_NC = None


def kernel(**inputs):
    global _NC
    if _NC is None:
        _NC = build()
    maps = make_in_maps(**inputs)
    res = run_bass_kernel_spmd(_NC, maps, core_ids=list(range(8)))
    out = np.empty((8, NT, 1024), dtype=np.float32)
    for b in range(8):
        out[b] = np.asarray(res.results[b]["outT"]).reshape(1024, NT).T
    return out
```

```python
import numpy as np
from contextlib import ExitStack
import concourse.bass as bass
import concourse.mybir as mybir
from concourse.bass_utils import run_bass_kernel_spmd

F32 = mybir.dt.float32
BF16 = mybir.dt.bfloat16
U32 = mybir.dt.uint32
ALU = mybir.AluOpType
AF = mybir.ActivationFunctionType
AX = mybir.AxisListType

T = 4112
NT = 4096
NMETA = 16
EPS = 1e-6
ENG = ['pe', 'act', 'dve', 'pool', 'sp']
SEM_LIMIT = 30000


class Tok:
    __slots__ = ('sem', 'val', 'eng', 'dma')

    def __init__(self, sem, val, eng, dma):
        self.sem = sem; self.val = val; self.eng = eng; self.dma = dma


class Prog:
    def __init__(self, nc, stack):
        self.nc = nc; self.stack = stack
        self.ops = {e: [] for e in ENG}
        self.esem = {}; self.ecnt = {}
        self.waited = {e: {} for e in ENG}
        self.lastw = {}; self.readers = {}
        self.dsem = {}
        self.nsem = 0
        self.allsems = {}
        self.rr = 0
        self.bg = set()

    def newsem(self):
        self.nsem += 1
        s = self.stack.enter_context(self.nc.semaphore(f"s{self.nsem}"))
        self.allsems[id(s)] = [s, 0]
        return s

    def _tok(self, eng, dma_key):
        if dma_key is None:
            if eng not in self.esem or self.ecnt[eng] >= SEM_LIMIT:
                self.esem[eng] = self.newsem(); self.ecnt[eng] = 0
            self.ecnt[eng] += 1
            t = Tok(self.esem[eng], self.ecnt[eng], eng, False)
        else:
            if dma_key not in self.dsem or self.dsem[dma_key][1] >= SEM_LIMIT:
                self.dsem[dma_key] = [self.newsem(), 0]
            self.dsem[dma_key][1] += 16
            t = Tok(self.dsem[dma_key][0], self.dsem[dma_key][1], eng, True)
        self.allsems[id(t.sem)][1] = t.val
        return t

    def add(self, eng, fn, reads=(), writes=(), dma_key=None):
        need = {}

        def want(t, kind):
            if (not t.dma) and t.eng == eng and dma_key is None and eng == 'pe':
                return
            k = id(t.sem)
            if k not in need or need[k][1] < t.val:
                need[k] = (t.sem, t.val)
        for k in reads:
            t = self.lastw.get(k)
            if t is not None:
                want(t, 'raw')
        for k in writes:
            t = self.lastw.get(k)
            if t is not None:
                want(t, 'waw')
            for r in self.readers.get(k, {}).values():
                want(r, 'war')
        waits = []
        wd = self.waited[eng]
        for k, (sem, val) in need.items():
            if wd.get(k, 0) >= val:
                continue
            wd[k] = val
            waits.append((sem, val))
        tok = self._tok(eng, dma_key)
        inc = 16 if dma_key is not None else 1

        def emit(e):
            for s, v in waits[1:]:
                e.wait_ge(s, v)
            inst = fn(e)
            if waits:
                inst._wait_ge(waits[0][0], waits[0][1])
            inst.then_inc(tok.sem, inc)
        self.ops[eng].append(emit)
        for k in reads:
            self.readers.setdefault(k, {})[id(tok.sem)] = tok
        for k in writes:
            self.lastw[k] = tok
            self.readers[k] = {}
        return tok

    def barrier(self):
        for e in ENG:
            wd = self.waited[e]
            for k, (s, v) in self.allsems.items():
                if k in self.bg:
                    continue
                if v > 0 and wd.get(k, 0) < v:
                    wd[k] = v
                    self.ops[e].append(lambda en, s=s, v=v: en.wait_ge(s, v))
        self.lastw = {}; self.readers = {}

    def flush(self):
        ops = self.ops
        with self.nc.Block() as block:
            @block.sync
            def _(e):
                for f in ops['sp']:
                    f(e)

            @block.tensor
            def _(e):
                for f in ops['pe']:
                    f(e)

            @block.vector
            def _(e):
                for f in ops['dve']:
                    f(e)

            @block.scalar
            def _(e):
                for f in ops['act']:
                    f(e)

            @block.gpsimd
            def _(e):
                for f in ops['pool']:
                    f(e)
        self.ops = {e: [] for e in ENG}

    def mm(self, out, lhsT, rhs, start, stop, r, w):
        return self.add('pe', lambda e: e.matmul(out, lhsT, rhs, start=start, stop=stop), r, w)

    def tr(self, out, in_, ident, r, w):
        return self.add('pe', lambda e: e.transpose(out, in_, ident), r, w)

    def act(self, out, in_, func, r, w, scale=1.0, bias=0.0):
        return self.add('act', lambda e: e.activation(out=out, in_=in_, func=func, bias=bias, scale=scale), r, w)

    def tt(self, eng, out, in0, in1, op, r, w):
        return self.add(eng, lambda e: e.tensor_tensor(out=out, in0=in0, in1=in1, op=op), r, w)

    def ts(self, eng, out, in0, s1, s2, op0, op1, r, w):
        if op1 is None:
            return self.add(eng, lambda e: e.tensor_scalar(out=out, in0=in0, scalar1=s1, scalar2=None, op0=op0), r, w)
        return self.add(eng, lambda e: e.tensor_scalar(out=out, in0=in0, scalar1=s1, scalar2=s2, op0=op0, op1=op1), r, w)

    def stt(self, eng, out, in0, scalar, in1, op0, op1, r, w):
        return self.add(eng, lambda e: e.scalar_tensor_tensor(out=out, in0=in0, scalar=scalar, in1=in1, op0=op0, op1=op1), r, w)

    def copy(self, eng, out, in_, r, w):
        if eng == 'act':
            return self.add('act', lambda e: e.copy(out=out, in_=in_), r, w)
        return self.add(eng, lambda e: e.tensor_copy(out=out, in_=in_), r, w)

    def dma(self, eng, out, in_, r, w, key):
        return self.add(eng, lambda e: e.dma_start(out=out, in_=in_), r, w, dma_key=key)

    def rsqrt_mean(self, out, in_, n, r, w, tmpkey=None):
        self.act(out, in_, AF.Sqrt, r, w, scale=1.0 / n, bias=EPS)
        self.add('dve', lambda e: e.reciprocal(out=out, in_=out), w, w)


def build(debug=False, phases="0ABCDE"):
    nc = bass.Bass("TRN2", target_bir_lowering=False)

    def din(name, shape, dt=F32):
        return nc.dram_tensor(name, list(shape), dt, kind="ExternalInput").ap()

    def dscr(name, shape, dt):
        if debug:
            return nc.dram_tensor(name, list(shape), dt, kind="ExternalOutput").ap()
        return nc.dram_tensor(name, list(shape), dt).ap()

    hT0 = din("hT0", [8, 128, T])
    w_in = din("w_in", [8, 128, 4608])
    mixw = din("mixw", [128, 8])
    cosT = din("cosT", [128, T])
    sinT = din("sinT", [128, T])
    lbl_f = din("lbl_f", [128, 8])
    lbl_t = din("lbl_t", [1, 1024])
    recnw = din("recnw", [128, 4])
    sublnw = din("sublnw", [128, 1])
    lamp = din("lamp", [1, 256])
    w_out = din("w_out", [8, 128, 1024])
    ffnw = din("ffnw", [128, 8])
    finw = din("finw", [128, 8])
    wq = din("wq", [8, 128, 2048])
    skT = din("skT", [128, 2048])
    u2 = din("u2", [128, 128, 1024])
    v2 = din("v2", [128, 128, 1024])
    c_ident = din("c_ident", [128, 128])
    c_U = din("c_U", [64, 64])
    c_L = din("c_L", [64, 64])
    c_mask = din("c_mask", [128, 2048])
    c_iota = din("c_iota", [128, 128])

    outT = nc.dram_tensor("outT", [8, 128, NT], F32, kind="ExternalOutput").ap()

    qT_s = dscr("qT_s", [4, 128, T], BF16)
    kT_s = dscr("kT_s", [4, 128, T], BF16)
    v_s = dscr("v_s", [T, 512], BF16)
    rqT_s = dscr("rqT_s", [4, 128, T], F32)
    rkT_s = dscr("rkT_s", [4, 128, T], F32)
    rgT_s = dscr("rgT_s", [4, 128, T], F32)
    rk_s = dscr("rk_s", [T, 512], F32)
    rg_s = dscr("rg_s", [T, 512], F32)
    ri_s = dscr("ri_s", [T, 512], BF16)
    mixT_s = dscr("mixT_s", [8, 128, T], BF16)
    h1T_s = dscr("h1T_s", [8, 128, T], F32)
    hnT_s = dscr("hnT_s", [8, 128, T], BF16)
    u2b = nc.dram_tensor("u2b", [128, 128, 1024], BF16).ap()
    v2b = nc.dram_tensor("v2b", [128, 128, 1024], BF16).ap()
    Gs = nc.dram_tensor("Gs", [32, 128, 128 * 128], BF16).ap()

    blocks = [(i * 512, 512) for i in range(8)] + [(4096, 16)]

    with ExitStack() as top:
        P = Prog(nc, top)

        def SB(st, name, shape, dt):
            return st.enter_context(nc.sbuf_tensor(name, list(shape), dt))

        def PS(st, name, shape, dt=F32):
            return st.enter_context(nc.psum_tensor(name, list(shape), dt))

        ones_f = SB(top, "ones_f", [128, 128], F32)
        ones_b = SB(top, "ones_b", [128, 128], BF16)
        ident = SB(top, "ident", [128, 128], F32)
        P.add('dve', lambda e: e.memset(ones_f[:], 1.0), (), ['ones_f'])
        P.add('dve', lambda e: e.memset(ones_b[:], 1.0), (), ['ones_b'])
        P.dma('sp', ident[:], c_ident, (), ['ident'], 'ident')

        if 'A' in phases:
            with ExitStack() as st:
                aT = SB(st, "aT", [128, 8, 1024], BF16)
                wb = SB(st, "wb", [128, 8, 4608], BF16)
                mw = SB(st, "mw", [128, 8], F32)
                lbf = SB(st, "lbf", [128, 8], F32)
                omlf = SB(st, "omlf", [128, 4], F32)
                lbt = SB(st, "lbt", [128, 1024], F32)
                omlt = SB(st, "omlt", [128, 512], F32)
                P.dma('sp', mw[:], mixw, (), ['mw'], 'mw')
                P.dma('sp', lbf[:], lbl_f, (), ['lbf'], 'lbf')
                P.dma('sp', lbt[:], lbl_t.partition_broadcast(128), (), ['lbt'], 'lbt')
                P.tt('dve', omlf[:], lbf[:, 4:8], lbf[:, 0:4], ALU.subtract, ['lbf'], ['omlf'])
                P.act(omlf[:], omlf[:], AF.Sigmoid, ['omlf'], ['omlf'])
                P.tt('dve', omlt[:], lbt[:, 512:1024], lbt[:, 0:512], ALU.subtract, ['lbt'], ['omlt'])
                P.act(omlt[:], omlt[:], AF.Sigmoid, ['omlt'], ['omlt'])
                for cg in range(9):
                    P.dma('pool', wb[:, :, cg * 512:(cg + 1) * 512], w_in[:, :, cg * 512:(cg + 1) * 512].rearrange("k p c -> p k c"),
                          (), ['wb'], 'wb')
                bg_list = []
                if '0' in phases:
                    for (src, dst, key) in ((u2, u2b, 'p0u'), (v2, v2b, 'p0v')):
                        for c0 in range(0, 128, 8):
                            bg_list.append((dst[c0:c0 + 8], src[c0:c0 + 8], key))

                def emit_bg(n):
                    for _ in range(n):
                        if bg_list:
                            d_, s_, k_ = bg_list.pop(0)
                            t = P.dma('pool', d_, s_, (), [], k_)
                            P.bg.add(id(t.sem))
                with ExitStack() as st2:
                    cs = [SB(st2, f"cs{i}", [128, 2, 512], F32) for i in range(2)]
                    NSTG = 6
                    stg = [SB(st2, f"stg{i}", [128, 512], F32) for i in range(NSTG)]
                    stgb = [SB(st2, f"stgb{i}", [128, 512], BF16) for i in range(NSTG)]
                    tmp = [SB(st2, f"tmp{i}", [128, 512], F32) for i in range(4)]
                    pp = [PS(st2, f"pp{i}", [128, 512]) for i in range(6)]
                    cnt = {'pp': 0, 'stg': 0, 'stgb': 0, 'tmp': 0}
                    xt = SB(st2, "xt0", [128, 8, 512], F32)
                    sq = SB(st2, "sq", [128, 8, 512], F32)
                    rstd = SB(st2, "rstd", [128, 512], F32)
                    ssp = PS(st2, "ssp", [128, 512])

                    def aoff(t):
                        return ((t // 512) % 2) * 512 + (t % 512)

                    def akey(t):
                        return f'aT{(t // 512) % 2}'

                    def norm(bi):
                        t0, n = blocks[bi]
                        P.dma('sp', xt[:, :, :n], hT0[:, :, t0:t0 + n].rearrange("k p t -> p k t"), (), ['xt0'], 'xt0')
                        P.act(sq[:, :, :n], xt[:, :, :n], AF.Square, ['xt0'], ['sq'])
                        for k in range(8):
                            P.mm(ssp[:, :n], ones_f[:], sq[:, k, :n], k == 0, k == 7, ['ones_f', 'sq'], ['ssp'])
                        P.rsqrt_mean(rstd[:, :n], ssp[:, :n], 1024.0, ['ssp'], ['rstd'])
                        o = aoff(t0)
                        for k in range(8):
                            P.stt('dve', aT[:, k, o:o + n], xt[:, k, :n], mw[:, k:k + 1], rstd[:, :n],
                                  ALU.mult, ALU.mult, ['xt0', 'mw', 'rstd'], [akey(t0)])

                    def nxt(kind, n):
                        cnt[kind] += 1
                        return cnt[kind] % n

                    def proj_f(pi, col0, t0, n):
                        for k in range(8):
                            P.mm(pp[pi][:, :n], wb[:, k, col0:col0 + 128], aT[:, k, aoff(t0):aoff(t0) + n], k == 0, k == 7,
                                 ['wb', akey(t0)], [f'pp{pi}'])

                    def proj_t(pi, col0, t0, r):
                        for k in range(8):
                            P.mm(pp[pi][:r, :], aT[:, k, aoff(t0):aoff(t0) + r], wb[:, k, col0:col0 + 512], k == 0, k == 7,
                                 ['wb', akey(t0)], [f'pp{pi}'])

                    norm(0)
                    for bi, (t0, n) in enumerate(blocks):
                        c = bi % 2
                        emit_bg(4)
                        P.dma('sp', cs[c][:, 0, :n], cosT[:, t0:t0 + n], (), [f'cs{c}'], f'cs{c}')
                        P.dma('sp', cs[c][:, 1, :n], sinT[:, t0:t0 + n], (), [f'cs{c}'], f'cs{c}')
                        for gi, (base, dst) in enumerate(((0, qT_s), (1024, kT_s))):
                            for h in range(4):
                                pa = nxt('pp', 6); proj_f(pa, base + h * 128, t0, n)
                                pb = nxt('pp', 6); proj_f(pb, base + 512 + h * 128, t0, n)
                                t1 = nxt('tmp', 4)
                                P.tt('dve', tmp[t1][:, :n], pp[pa][:, :n], cs[c][:, 0, :n], ALU.mult, [f'pp{pa}', f'cs{c}'], [f'tmp{t1}'])
                                t2 = nxt('tmp', 4)
                                P.tt('dve', tmp[t2][:, :n], pp[pb][:, :n], cs[c][:, 1, :n], ALU.mult, [f'pp{pb}', f'cs{c}'], [f'tmp{t2}'])
                                sb_ = nxt('stgb', NSTG)
                                P.tt('pool', stgb[sb_][:, :n], tmp[t1][:, :n], tmp[t2][:, :n], ALU.add, [f'tmp{t1}', f'tmp{t2}'], [f'stgb{sb_}'])
                                P.dma('pool', dst[h, :, t0:t0 + n], stgb[sb_][:, :n], [f'stgb{sb_}'], [], f'stgb{sb_}')
                        if bi + 1 < len(blocks):
                            norm(bi + 1)
                        for (base, dst) in ((2560, rqT_s), (4096, rgT_s)):
                            for h in range(4):
                                pa = nxt('pp', 6); proj_f(pa, base + h * 128, t0, n)
                                s_ = nxt('stg', NSTG)
                                P.act(stg[s_][:, :n], pp[pa][:, :n], AF.Silu, [f'pp{pa}'], [f'stg{s_}'])
                                P.dma('pool', dst[h, :, t0:t0 + n], stg[s_][:, :n], [f'stg{s_}'], [], f'stg{s_}')
                        for h in range(4):
                            pa = nxt('pp', 6); proj_f(pa, 3072 + h * 128, t0, n)
                            t1 = nxt('tmp', 4)
                            P.act(tmp[t1][:, :n], pp[pa][:, :n], AF.Sigmoid, [f'pp{pa}'], [f'tmp{t1}'], scale=-1.0)
                            s_ = nxt('stg', NSTG)
                            P.ts('pool', stg[s_][:, :n], tmp[t1][:, :n], omlf[:, h:h + 1], None, ALU.mult, None, [f'tmp{t1}', 'omlf'], [f'stg{s_}'])
                            P.dma('pool', rkT_s[h, :, t0:t0 + n], stg[s_][:, :n], [f'stg{s_}'], [], f'stg{s_}')
                        for r0 in range(0, n, 128):
                            r = min(128, n - r0)
                            tt0 = t0 + r0
                            for (base, dst) in ((2048, v_s), (3584, ri_s)):
                                pa = nxt('pp', 6); proj_t(pa, base, tt0, r)
                                sb_ = nxt('stgb', NSTG)
                                P.copy('act', stgb[sb_][:r, :], pp[pa][:r, :], [f'pp{pa}'], [f'stgb{sb_}'])
                                P.dma('pool', dst[tt0:tt0 + r, :], stgb[sb_][:r, :], [f'stgb{sb_}'], [], f'stgb{sb_}')
                            pa = nxt('pp', 6); proj_t(pa, 3072, tt0, r)
                            t1 = nxt('tmp', 4)
                            P.act(tmp[t1][:r, :], pp[pa][:r, :], AF.Sigmoid, [f'pp{pa}'], [f'tmp{t1}'], scale=-1.0)
                            s_ = nxt('stg', NSTG)
                            P.tt('pool', stg[s_][:r, :], tmp[t1][:r, :], omlt[:r, :], ALU.mult, [f'tmp{t1}', 'omlt'], [f'stg{s_}'])
                            P.dma('pool', rk_s[tt0:tt0 + r, :], stg[s_][:r, :], [f'stg{s_}'], [], f'stg{s_}')
                            s2 = nxt('stg', NSTG)
                            P.act(stg[s2][:r, :], stg[s_][:r, :], AF.Ln, [f'stg{s_}'], [f'stg{s2}'], scale=-1.0, bias=1.0)
                            P.dma('pool', rg_s[tt0:tt0 + r, :], stg[s2][:r, :], [f'stg{s2}'], [], f'stg{s2}')
                    emit_bg(64)
                    P.barrier(); P.flush()

        if 'B' in phases:
            with ExitStack() as st:
                kTh = [SB(st, f"kTh{i}", [64, 2, T], BF16) for i in range(2)]
                vh = [SB(st, f"vh{i}", [128, 33, 128], BF16) for i in range(2)]
                mskf = SB(st, "mskf", [128, 2048], F32)
                msk = SB(st, "msk", [128, 4, 512], BF16)
                qb_ = [SB(st, f"qb{i}", [64, 2, 512], BF16) for i in range(2)]
                pb_ = [SB(st, f"pb{i}", [128, 512], BF16) for i in range(8)]
                lam4 = SB(st, "lam4", [128, 256], F32)
                lamt = SB(st, "lamt", [128, 64], F32)
                lam2 = SB(st, "lam2", [128, 2], F32)
                neglam = SB(st, "neglam", [128, 1], F32)
                sw = SB(st, "sw", [128, 1], F32)
                rr = [SB(st, f"rr{i}", [128, 512], F32) for i in range(2)]
                to = [SB(st, f"to{i}", [128, 512], F32) for i in range(2)]
                oo = SB(st, "oo", [128, 512], F32)
                o2 = SB(st, "o2", [128, 512], F32)
                rs = SB(st, "rs", [128, 512], F32)
                ob = [SB(st, f"ob{i}", [128, 512], BF16) for i in range(2)]
                pS = [PS(st, f"pS{i}", [128, 512]) for i in range(4)]
                pO = [PS(st, f"pO{i}", [128, 512]) for i in range(2)]
                pL = [PS(st, f"pL{i}", [128, 512]) for i in range(2)]
                pN = pS[0]
                P.dma('sp', mskf[:], c_mask, (), ['mskf'], 'mskf')
                P.copy('dve', msk[:].rearrange("p a b -> p (a b)"), mskf[:], ['mskf'], ['msk'])
                P.dma('sp', lam4[:], lamp.partition_broadcast(128), (), ['lam4'], 'lam4')
                P.dma('sp', sw[:], sublnw, (), ['sw'], 'sw')
                P.ts('dve', sw[:], sw[:], 0.8, None, ALU.mult, None, ['sw'], ['sw'])
                for j in range(2):
                    P.tt('dve', lamt[:], lam4[:, j * 128:j * 128 + 64], lam4[:, j * 128 + 64:j * 128 + 128], ALU.mult, ['lam4'], ['lamt'])
                    P.add('dve', lambda e, j=j: e.reduce_sum(out=lam2[:, j:j + 1], in_=lamt[:], axis=AX.X), ['lamt'], ['lam2'])
                P.act(lam2[:], lam2[:], AF.Exp, ['lam2'], ['lam2'])
                P.tt('dve', neglam[:], lam2[:, 1:2], lam2[:, 0:1], ALU.subtract, ['lam2'], ['neglam'])
                P.ts('dve', neglam[:], neglam[:], -0.2, None, ALU.add, None, ['neglam'], ['neglam'])
                pbi = 0; qi = 0; oi = 0; pendB = []; pendF = []

                def fin_b(h, qs, nq, osl):
                    for comp in range(2):
                        P.add('dve', lambda e, comp=comp, nq=nq: e.reciprocal(out=rr[comp][:, :nq], in_=rr[comp][:, :nq]),
                              [f'rr{comp}'], [f'rr{comp}'])
                        P.tt('dve', to[comp][:, :nq], to[comp][:, :nq], rr[comp][:, :nq], ALU.mult, [f'to{comp}', f'rr{comp}'], [f'to{comp}'])
                    P.stt('dve', oo[:, :nq], to[1][:, :nq], neglam[:, 0:1], to[0][:, :nq], ALU.mult, ALU.add,
                          ['to0', 'to1', 'neglam'], ['oo'])
                    P.tt('dve', o2[:, :nq], oo[:, :nq], oo[:, :nq], ALU.mult, ['oo'], ['o2'])
                    P.mm(pN[:, :nq], ones_f[:], o2[:, :nq], True, True, ['ones_f', 'o2'], ['pS0'])
                    P.act(rs[:, :nq], pN[:, :nq], AF.Ln, ['pS0'], ['rs'], scale=1.0 / 128.0, bias=EPS)
                    P.act(rs[:, :nq], rs[:, :nq], AF.Exp, ['rs'], ['rs'], scale=-0.5)
                    P.stt('dve', ob[osl][:, :nq], oo[:, :nq], sw[:, 0:1], rs[:, :nq], ALU.mult, ALU.mult, ['oo', 'sw', 'rs'], [f'ob{osl}'])
                    P.dma('pool', mixT_s[h, :, qs:qs + nq], ob[osl][:, :nq], [f'ob{osl}'], ['mixT_s'], f'ob{osl}')

                def emit_ol(comp, kr, kt, psl, nq, nkt, hs):
                    P.mm(pO[comp][:, :nq], vh[hs][:kr, kt, :], pb_[psl][:kr, :nq], kt == 0, kt == nkt - 1,
                         [f'vh{hs}', f'pb{psl}'], [f'pO{comp}'])
                    P.mm(pL[comp][:, :nq], ones_b[:kr, :], pb_[psl][:kr, :nq], kt == 0, kt == nkt - 1,
                         ['ones_b', f'pb{psl}'], [f'pL{comp}'])

                for h in range(4):
                    hs = h % 2
                    P.dma('sp', kTh[hs][:], kT_s[h].rearrange("(c d) t -> d c t", c=2), ['kT_s'], [f'kTh{hs}'], f'kTh{hs}')
                    P.dma('sp', vh[hs][:, 0:32, :], v_s[0:4096, h * 128:(h + 1) * 128].rearrange("(tt p) d -> p tt d", p=128),
                          ['v_s'], [f'vh{hs}'], f'vh{hs}')
                    P.dma('sp', vh[hs][0:16, 32, :], v_s[4096:4112, h * 128:(h + 1) * 128], ['v_s'], [f'vh{hs}'], f'vh{hs}')
                    for (qs, nq) in blocks:
                        qi += 1; qsl = qi % 2
                        P.dma('sp', qb_[qsl][:, :, :nq], qT_s[h, :, qs:qs + nq].rearrange("(c d) t -> d c t", c=2),
                              ['qT_s'], [f'qb{qsl}'], f'qb{qsl}')
                        nkt = (qs + nq + 127) // 128
                        for kt in range(nkt):
                            kr = min(128, T - kt * 128)
                            for comp in range(2):
                                pbi += 1; sl = pbi % 4; psl = pbi % 8
                                P.mm(pS[sl][:kr, :nq], kTh[hs][:, comp, kt * 128:kt * 128 + kr], qb_[qsl][:, comp, :nq], True, True,
                                     [f'kTh{hs}', f'qb{qsl}'], [f'pS{sl}'])
                                P.act(pb_[psl][:kr, :nq], pS[sl][:kr, :nq], AF.Exp, [f'pS{sl}'], [f'pb{psl}'], scale=0.125)
                                if kt * 128 >= qs:
                                    j = (kt * 128 - qs) // 128
                                    P.tt('dve', pb_[psl][:kr, :nq], pb_[psl][:kr, :nq], msk[:kr, j, :nq], ALU.mult,
                                         [f'pb{psl}', 'msk'], [f'pb{psl}'])
                                pendB.append((comp, kr, kt, psl, nq, nkt, hs))
                            if len(pendB) > 2:
                                emit_ol(*pendB.pop(0)); emit_ol(*pendB.pop(0))
                            if pendF and (kt == min(3, nkt - 1)):
                                fin_b(*pendF.pop(0))
                        while pendB:
                            emit_ol(*pendB.pop(0))
                        for comp in range(2):
                            P.copy('dve', rr[comp][:, :nq], pL[comp][:, :nq], [f'pL{comp}'], [f'rr{comp}'])
                            P.copy('act', to[comp][:, :nq], pO[comp][:, :nq], [f'pO{comp}'], [f'to{comp}'])
                        oi += 1
                        pendF.append((h, qs, nq, oi % 2))
                while pendF:
                    fin_b(*pendF.pop(0))
                P.barrier(); P.flush()

        if 'C' in phases:
            with ExitStack() as st:
                U = SB(st, "U", [64, 64], F32)
                L = SB(st, "L", [64, 64], F32)
                nw = SB(st, "nw", [128, 4], F32)
                P.dma('sp', U[:], c_U, (), ['U'], 'U')
                P.dma('sp', L[:], c_L, (), ['L'], 'L')
                P.dma('sp', nw[:], recnw, (), ['nw'], 'nw')
                gtok = [SB(st, f"gtok{i}", [64, 8, 512], F32) for i in range(2)]
                ktok = [SB(st, f"ktok{i}", [64, 8, 512], F32) for i in range(2)]
                vtok = [SB(st, f"vtok{i}", [64, 8, 512], BF16) for i in range(2)]
                rqT = [SB(st, f"rqT{i}", [128, 4, 512], F32) for i in range(2)]
                rkT = [SB(st, f"rkT{i}", [128, 4, 512], F32) for i in range(2)]
                rgT = [SB(st, f"rgT{i}", [128, 4, 512], F32) for i in range(2)]
                E1 = SB(st, "E1", [128, 4, 512], F32)
                E2 = SB(st, "E2", [128, 512], F32)
                qe = SB(st, "qe", [128, 4, 512], BF16)
                ke = SB(st, "ke", [128, 4, 512], BF16)
                Ed = SB(st, "Ed", [64, 512], F32)
                kd = SB(st, "kd", [64, 8, 512], BF16)
                ATm = [SB(st, f"ATm{i}", [64, 64], BF16) for i in range(8)]
                S = [SB(st, f"S{i}", [128, 128], F32) for i in range(4)]
                Sb = [SB(st, f"Sb{i}", [128, 128], BF16) for i in range(4)]
                o2 = SB(st, "co2", [128, 512], F32)
                rs = SB(st, "crs", [128, 512], F32)
                rec = SB(st, "rec", [128, 512], F32)
                recb = [SB(st, f"recb{i}", [128, 512], BF16) for i in range(2)]
                pC = PS(st, "pC", [128, 512])
                pF = PS(st, "pF", [128, 512])
                pA = PS(st, "pA", [64, 8, 64])
                pOo = [PS(st, f"pOo{i}", [128, 512]) for i in range(4)]
                pSp = PS(st, "pSp", [128, 4, 128])
                for h in range(4):
                    P.add('dve', lambda e, h=h: e.memset(S[h][:], 0.0), (), [f'S{h}'])
                    P.add('pool', lambda e, h=h: e.memset(Sb[h][:], 0.0), (), [f'Sb{h}'])
                ri_ = 0
                for bi, (t0, n) in enumerate(blocks):
                    s = bi % 2
                    nch = (n + 63) // 64
                    cl = min(64, n)
                    P.dma('sp', gtok[s][:cl, :nch, :], rg_s[t0:t0 + n, :].rearrange("(ch p) c -> p ch c", p=cl), ['rg_s'], [f'gtok{s}'], f'gtok{s}')
                    P.dma('sp', ktok[s][:cl, :nch, :], rk_s[t0:t0 + n, :].rearrange("(ch p) c -> p ch c", p=cl), ['rk_s'], [f'ktok{s}'], f'ktok{s}')
                    P.dma('sp', vtok[s][:cl, :nch, :], ri_s[t0:t0 + n, :].rearrange("(ch p) c -> p ch c", p=cl), ['ri_s'], [f'vtok{s}'], f'vtok{s}')
                    P.dma('sp', rqT[s][:, :, :n], rqT_s[:, :, t0:t0 + n].rearrange("h p t -> p h t"), ['rqT_s'], [f'rqT{s}'], f'rqT{s}')
                    P.dma('sp', rkT[s][:, :, :n], rkT_s[:, :, t0:t0 + n].rearrange("h p t -> p h t"), ['rkT_s'], [f'rkT{s}'], f'rkT{s}')
                    P.dma('sp', rgT[s][:, :, :n], rgT_s[:, :, t0:t0 + n].rearrange("h p t -> p h t"), ['rgT_s'], [f'rgT{s}'], f'rgT{s}')
                    for h in range(4):
                        for ch in range(nch):
                            P.mm(pC[:, ch * 64:ch * 64 + cl], gtok[s][:cl, ch, h * 128:(h + 1) * 128], U[:cl, :cl], True, True,
                                 [f'gtok{s}', 'U'], ['pC'])
                        P.act(E1[:, h, :n], pC[:, :n], AF.Exp, ['pC'], [f'E1_{h}'])
                        P.act(E2[:, :n], pC[:, :n], AF.Exp, ['pC'], ['E2'], scale=-1.0)
                        P.tt('dve', qe[:, h, :n], rqT[s][:, h, :n], E1[:, h, :n], ALU.mult, [f'rqT{s}', f'E1_{h}'], [f'qe{h}'])
                        P.tt('pool', ke[:, h, :n], rkT[s][:, h, :n], E2[:, :n], ALU.mult, [f'rkT{s}', 'E2'], [f'ke{h}'])
                    for ch in range(nch):
                        P.mm(pF[:cl, :], L[:cl, :cl], gtok[s][:cl, ch, :], True, True, [f'gtok{s}', 'L'], ['pF'])
                        P.act(Ed[:cl, :], pF[:cl, :], AF.Exp, ['pF'], ['Ed'])
                        P.tt(('dve', 'pool')[ch % 2], kd[:cl, ch, :], ktok[s][:cl, ch, :], Ed[:cl, :], ALU.mult, [f'ktok{s}', 'Ed'], [f'kd{ch}'])
                    def stageA(ch):
                        c0 = ch * 64; st_ = ch % 2
                        for h in range(4):
                            P.mm(pA[:cl, st_ * 4 + h, :cl], ke[:, h, c0:c0 + cl], qe[:, h, c0:c0 + cl], True, True, [f'ke{h}', f'qe{h}'], ['pA'])

                    def stageA2(ch):
                        st_ = ch % 2
                        for h in range(4):
                            P.tt('dve', ATm[st_ * 4 + h][:cl, :cl], pA[:cl, st_ * 4 + h, :cl], U[:cl, :cl], ALU.mult, ['pA', 'U'], [f'ATm{st_}{h}'])

                    for ch in range(nch):
                        c0 = ch * 64; st_ = ch % 2
                        stageA(ch); stageA2(ch)
                        for h in range(4):
                            hc = slice(h * 128, (h + 1) * 128)
                            P.mm(pOo[h][:, c0:c0 + cl], Sb[h][:], qe[:, h, c0:c0 + cl], True, False, [f'Sb{h}', f'qe{h}'], [f'pOo{h}'])
                            P.mm(pOo[h][:, c0:c0 + cl], vtok[s][:cl, ch, hc], ATm[st_ * 4 + h][:cl, :cl], False, True, [f'vtok{s}', f'ATm{st_}{h}'], [f'pOo{h}'])
                            P.mm(pSp[:, h, :], kd[:cl, ch, hc], vtok[s][:cl, ch, hc], True, True, [f'kd{ch}', f'vtok{s}'], ['pSp'])
                        for h in range(4):
                            P.stt('dve', S[h][:], S[h][:], E1[:, h, c0 + cl - 1:c0 + cl], pSp[:, h, :], ALU.mult, ALU.add,
                                  [f'S{h}', f'E1_{h}', 'pSp'], [f'S{h}'])
                        for h in range(4):
                            P.copy('act', Sb[h][:], S[h][:], [f'S{h}'], [f'Sb{h}'])
                    for h in range(4):
                        P.act(o2[:, :n], pOo[h][:, :n], AF.Square, [f'pOo{h}'], ['co2'])
                        P.mm(pF[:, :n], ones_f[:], o2[:, :n], True, True, ['ones_f', 'co2'], ['pF'])
                        P.act(rs[:, :n], pF[:, :n], AF.Ln, ['pF'], ['crs'], scale=1.0 / 128.0, bias=EPS)
                        P.act(rs[:, :n], rs[:, :n], AF.Exp, ['crs'], ['crs'], scale=-0.5)
                        P.stt('dve', rec[:, :n], pOo[h][:, :n], nw[:, h:h + 1], rs[:, :n], ALU.mult, ALU.mult, [f'pOo{h}', 'nw', 'crs'], ['rec'])
                        ri_ += 1; rsl = ri_ % 2
                        P.tt('pool', recb[rsl][:, :n], rec[:, :n], rgT[s][:, h, :n], ALU.mult, ['rec', f'rgT{s}'], [f'recb{rsl}'])
                        P.dma('pool', mixT_s[4 + h, :, t0:t0 + n], recb[rsl][:, :n], [f'recb{rsl}'], ['mixT_s'], f'recb{rsl}')
                P.barrier(); P.flush()

        if 'D' in phases:
            with ExitStack() as st:
                wob = SB(st, "wob", [128, 8, 1024], BF16)
                fw = SB(st, "fw", [128, 8], F32)
                mixb = [SB(st, f"mixb{i}", [128, 8, 512], BF16) for i in range(2)]
                xt = [SB(st, f"dxt{i}", [128, 8, 512], F32) for i in range(2)]
                h1 = [SB(st, f"h1_{i}", [128, 8, 512], F32) for i in range(2)]
                sq = SB(st, "dsq", [128, 8, 512], F32)
                rstd = SB(st, "drstd", [128, 512], F32)
                hn = [SB(st, f"hn{i}", [128, 8, 512], BF16) for i in range(2)]
                pp = [PS(st, f"dpp{i}", [128, 512]) for i in range(4)]
                ssp = PS(st, "dssp", [128, 512])
                P.dma('sp', fw[:], ffnw, (), ['fw'], 'fw')
                for cg in range(2):
                    P.dma('pool', wob[:, :, cg * 512:(cg + 1) * 512], w_out[:, :, cg * 512:(cg + 1) * 512].rearrange("k p c -> p k c"),
                          (), ['wob'], 'wob')
                pi = [0]

                def d_proj(bi):
                    t0, n = blocks[bi]; s = bi % 2
                    P.dma('sp', mixb[s][:, :, :n], mixT_s[:, :, t0:t0 + n].rearrange("k p t -> p k t"), ['mixT_s'], [f'mixb{s}'], f'mixb{s}')
                    P.dma('sp', xt[s][:, :, :n], hT0[:, :, t0:t0 + n].rearrange("k p t -> p k t"), (), [f'dxt{s}'], f'dxt{s}')
                    for m in range(8):
                        pi[0] += 1; ps_ = pi[0] % 4
                        for k in range(8):
                            P.mm(pp[ps_][:, :n], wob[:, k, m * 128:(m + 1) * 128], mixb[s][:, k, :n], k == 0, k == 7,
                                 ['wob', f'mixb{s}'], [f'dpp{ps_}'])
                        P.tt('dve', h1[s][:, m, :n], pp[ps_][:, :n], xt[s][:, m, :n], ALU.add, [f'dpp{ps_}', f'dxt{s}'], [f'h1_{s}'])
                    P.dma('pool', h1T_s[:, :, t0:t0 + n].rearrange("k p t -> p k t"), h1[s][:, :, :n], [f'h1_{s}'], ['h1T_s'], f'h1_{s}')

                def d_post(bi):
                    t0, n = blocks[bi]; s = bi % 2
                    P.act(sq[:, :, :n], h1[s][:, :, :n], AF.Square, [f'h1_{s}'], ['dsq'])
                    for k in range(8):
                        P.mm(ssp[:, :n], ones_f[:], sq[:, k, :n], k == 0, k == 7, ['ones_f', 'dsq'], ['dssp'])
                    P.rsqrt_mean(rstd[:, :n], ssp[:, :n], 1024.0, ['dssp'], ['drstd'])
                    for k in range(8):
                        P.stt('dve', hn[s][:, k, :n], h1[s][:, k, :n], fw[:, k:k + 1], rstd[:, :n], ALU.mult, ALU.mult,
                              [f'h1_{s}', 'fw', 'drstd'], [f'hn{s}'])
                    P.dma('pool', hnT_s[:, :, t0:t0 + n].rearrange("k p t -> p k t"), hn[s][:, :, :n], [f'hn{s}'], ['hnT_s'], f'hn{s}')

                d_proj(0)
                for bi in range(len(blocks)):
                    if bi + 1 < len(blocks):
                        d_proj(bi + 1)
                    d_post(bi)
                P.barrier(); P.flush()

        if any(c in phases for c in 'Eab'):
            with ExitStack() as st:
                wqb = SB(st, "wqb", [128, 8, 2048], BF16)
                skb = SB(st, "skb", [128, 16, 128], BF16)
                iota = SB(st, "iota", [128, 128], F32)
                P.dma('sp', iota[:], c_iota, (), ['iota'], 'iota')
                for cg in range(4):
                    P.dma('pool', wqb[:, :, cg * 512:(cg + 1) * 512], wq[:, :, cg * 512:(cg + 1) * 512].rearrange("k p c -> p k c"),
                          (), ['wqb'], 'wqb')
                P.dma('pool', skb[:].rearrange("p a b -> p (a b)"), skT, (), ['skb'], 'skb')
                hnt4 = SB(st, "hnt4", [128, 8, 512], BF16)
                qpT4 = SB(st, "qpT4", [128, 16, 512], BF16)
                ssbs = [SB(st, f"ssb{i}", [128, 16, 128], F32) for i in range(2)]
                wk = SB(st, "wk", [128, 128], F32)
                sv = SB(st, "sv", [128, 16, 16], F32)
                si = SB(st, "si", [128, 16, 16], U32)
                sif = SB(st, "sif", [128, 16, 16], F32)
                cand = SB(st, "cand", [128, 8, 256], F32)
                wk2 = SB(st, "wk2", [128, 256], F32)
                tv = SB(st, "tv", [128, 8, 16], F32)
                pos = SB(st, "pos", [128, 8, 16], U32)
                au = SB(st, "au", [128, 8, 16], U32)
                bu = SB(st, "bu", [128, 8, 16], U32)
                af = SB(st, "af", [128, 8, 16], F32)
                bf = SB(st, "bf", [128, 8, 16], F32)
                ex = SB(st, "ex", [128, 8, 16], F32)
                zz = SB(st, "zz", [128, 8], F32)
                gate = SB(st, "gate", [128, 8, 16], F32)
                eq = SB(st, "eq", [128, 8, 16, 16], F32)
                eq2 = SB(st, "eq2", [128, 8, 16, 16], F32)
                idi = SB(st, "idi", [128, 8, 16], F32)
                idj = SB(st, "idj", [128, 8, 16], F32)
                tTs = [SB(st, f"tT{i}", [128, 3, 128], F32) for i in range(2)]
                OJ = [SB(st, f"OJ{i}", [128, 32, 128], BF16) for i in range(2)]
                OI = [SB(st, f"OI{i}", [128, 32, 128], BF16) for i in range(2)]
                iotab = SB(st, "iotab", [128, 128], BF16)
                iota3b = SB(st, "iota3b", [128, 32, 128], BF16)
                P.copy('dve', iotab[:], iota[:], ['iota'], ['iotab'])
                P.copy('dve', iota3b[:], iotab[:].unsqueeze(1).to_broadcast([128, 32, 128]), ['iotab'], ['iota3b'])
                Gst = [SB(st, f"Gst{i}", [128, 128, 128], BF16) for i in range(1)]
                pq = [PS(st, f"pq{i}", [128, 512]) for i in range(2)]
                psc = [PS(st, f"psc{i}", [128, 512]) for i in range(2)]
                pT = PS(st, "pT", [128, 3, 128])
                pG = [PS(st, f"pG{i}", [128, 128, 4]) for i in range(3)]
                gi = [0]
                do_a = ('E' in phases or 'a' in phases)

                def s1big(q4):
                    p0 = NMETA + q4 * 512
                    P.dma('sp', hnt4[:], hnT_s[:, :, p0:p0 + 512].rearrange("k p t -> p k t"), ['hnT_s'], ['hnt4'], 'hnt4')
                    for ch in range(16):
                        b_ = ch % 2
                        for k in range(8):
                            P.mm(pq[b_][:], wqb[:, k, ch * 128:(ch + 1) * 128], hnt4[:, k, :], k == 0, k == 7, ['wqb', 'hnt4'], [f'pq{b_}'])
                        P.copy('act', qpT4[:, ch, :], pq[b_][:], [f'pq{b_}'], ['qpT4'])

                def s1(ti):
                    s = ti % 2
                    off = (ti % 4) * 128
                    for g4 in range(4):
                        b_ = g4 % 2
                        for c4 in range(4):
                            ch = g4 * 4 + c4
                            P.mm(psc[b_][:, c4 * 128:(c4 + 1) * 128], qpT4[:, ch, off:off + 128], skb[:, ch, :], True, True, ['qpT4', 'skb'], [f'psc{b_}'])
                        P.copy('act', ssbs[s][:, g4 * 4:(g4 + 1) * 4, :].rearrange("p a b -> p (a b)"), psc[b_][:], [f'psc{b_}'], [f'ssb{s}'])

                def s2_parts(ti):
                    s = ti % 2
                    ssb = ssbs[s]; sk = f'ssb{s}'
                    sv4 = sv[:].rearrange("p (h two) k -> p h two k", two=2)
                    sif4 = sif[:].rearrange("p (h two) k -> p h two k", two=2)

                    def lvl1(g0, g1):
                        for g in range(g0, g1):
                            P.add('dve', lambda e, g=g: e.max(out=sv[:, g, 0:8], in_=ssb[:, g, :]), [sk], ['sv'])
                            P.add('dve', lambda e, g=g: e.max_index(out=si[:, g, 0:8], in_max=sv[:, g, 0:8], in_values=ssb[:, g, :]), [sk, 'sv'], ['si'])
                            P.add('dve', lambda e, g=g: e.match_replace(out=wk[:], in_to_replace=sv[:, g, 0:8], in_values=ssb[:, g, :], imm_value=-1e30),
                                  [sk, 'sv'], ['wk'])
                            P.add('dve', lambda e, g=g: e.max(out=sv[:, g, 8:16], in_=wk[:]), ['wk'], ['sv'])
                            P.add('dve', lambda e, g=g: e.max_index(out=si[:, g, 8:16], in_max=sv[:, g, 8:16], in_values=wk[:]), ['wk', 'sv'], ['si'])

                    def pre():
                        lvl1(0, 4)

                    def part0():
                        lvl1(4, 8)

                    def part1():
                        lvl1(8, 16)
                        P.copy('pool', sif[:], si[:], ['si'], ['sif'])
                        P.tt('pool', cand[:].rearrange("p h (a b) -> p h a b", a=16),
                             sv4[:, :, 0, :].unsqueeze(3).to_broadcast([128, 8, 16, 16]),
                             sv4[:, :, 1, :].unsqueeze(2).to_broadcast([128, 8, 16, 16]), ALU.add, ['sv'], ['cand'])

                    def part2():
                        for h in range(8):
                            P.add('dve', lambda e, h=h: e.max(out=tv[:, h, 0:8], in_=cand[:, h, :]), ['cand'], ['tv'])
                            P.add('dve', lambda e, h=h: e.max_index(out=pos[:, h, 0:8], in_max=tv[:, h, 0:8], in_values=cand[:, h, :]), ['cand', 'tv'], ['pos'])
                            P.add('dve', lambda e, h=h: e.match_replace(out=wk2[:], in_to_replace=tv[:, h, 0:8], in_values=cand[:, h, :], imm_value=-1e30),
                                  ['cand', 'tv'], ['wk2'])
                            P.add('dve', lambda e, h=h: e.max(out=tv[:, h, 8:16], in_=wk2[:]), ['wk2'], ['tv'])
                            P.add('dve', lambda e, h=h: e.max_index(out=pos[:, h, 8:16], in_max=tv[:, h, 8:16], in_values=wk2[:]), ['wk2', 'tv'], ['pos'])

                    def part3():
                        P.tt('pool', ex[:], tv[:], tv[:, :, 0:1].to_broadcast([128, 8, 16]), ALU.subtract, ['tv'], ['ex'])
                        P.act(ex[:], ex[:], AF.Exp, ['ex'], ['ex'])
                        P.add('dve', lambda e: e.reduce_sum(out=zz[:], in_=ex[:], axis=AX.X), ['ex'], ['zz'])
                        P.add('dve', lambda e: e.reciprocal(out=zz[:], in_=zz[:]), ['zz'], ['zz'])
                        P.tt('pool', gate[:], ex[:], zz[:].unsqueeze(2).to_broadcast([128, 8, 16]), ALU.mult, ['ex', 'zz'], ['gate'])
                        P.add('dve', lambda e: e.tensor_single_scalar(out=au[:], in_=pos[:], scalar=4, op=ALU.logical_shift_right), ['pos'], ['au'])
                        P.add('dve', lambda e: e.tensor_single_scalar(out=bu[:], in_=pos[:], scalar=15, op=ALU.bitwise_and), ['pos'], ['bu'])
                        P.copy('pool', af[:], au[:], ['au'], ['af'])
                        P.copy('pool', bf[:], bu[:], ['bu'], ['bf'])
                        io16 = iota[:, 0:16].unsqueeze(1).unsqueeze(1).to_broadcast([128, 8, 16, 16])
                        for (src, half, dst, nm, eqt, ek) in ((af, 0, idi, 'idi', eq, 'eq'), (bf, 1, idj, 'idj', eq2, 'eq2')):
                            P.tt('dve', eqt[:], src[:].unsqueeze(3).to_broadcast([128, 8, 16, 16]), io16, ALU.is_equal, [('af', 'bf')[half], 'iota'], [ek])
                            P.tt('dve', eqt[:], eqt[:], sif4[:, :, half, :].unsqueeze(2).to_broadcast([128, 8, 16, 16]), ALU.mult, [ek, 'sif'], [ek])
                        for (dst, nm, eqt, ek) in ((idi, 'idi', eq, 'eq'), (idj, 'idj', eq2, 'eq2')):
                            P.add('dve', lambda e, dst=dst, eqt=eqt: e.reduce_sum(out=dst[:], in_=eqt[:], axis=AX.X), [ek], [nm])
                        for j, (src, nm) in enumerate(((idi, 'idi'), (idj, 'idj'), (gate, 'gate'))):
                            P.tr(pT[:, j, :], src[:].rearrange("p h k -> p (h k)"), ident[:], [nm, 'ident'], ['pT'])
                        P.copy('act', tTs[s][:], pT[:], ['pT'], [f'tT{s}'])

                    return [pre, part0, part1, part2, part3]

                def s3(ti, parts):
                    s = ti % 2
                    tT = tTs[s]; tk = f'tT{s}'
                    gs = 0

                    def expand(pc):
                        half = pc % 2; n0 = pc * 32
                        P.act(OJ[half][:], tT[:, 1, n0:n0 + 32].unsqueeze(2).to_broadcast([128, 32, 128]), AF.Copy, [tk], [f'OJ{half}'])
                        P.act(OI[half][:], tT[:, 0, n0:n0 + 32].unsqueeze(2).to_broadcast([128, 32, 128]), AF.Copy, [tk], [f'OI{half}'])

                    def onehot(pc):
                        half = pc % 2; n0 = pc * 32
                        P.tt('dve', OJ[half][:], OJ[half][:], iota3b[:], ALU.is_equal, [f'OJ{half}', 'iota3b'], [f'OJ{half}'])
                        P.tt('dve', OI[half][:], OI[half][:], iota3b[:], ALU.is_equal, [f'OI{half}', 'iota3b'], [f'OI{half}'])
                        P.tt('pool', OI[half][:], OI[half][:], tT[:, 2, n0:n0 + 32].unsqueeze(2).to_broadcast([128, 32, 128]), ALU.mult,
                             [f'OI{half}', tk], [f'OI{half}'])

                    def gmm(pc):
                        half = pc % 2; n0 = pc * 32
                        for n4 in range(0, 32, 4):
                            gi[0] += 1; gb = gi[0] % 3
                            for q in range(4):
                                nn = n4 + q
                                P.mm(pG[gb][:, :, q], OI[half][:, nn, :], OJ[half][:, nn, :], True, True, [f'OI{half}', f'OJ{half}'], [f'pG{gb}'])
                            dstap = Gst[gs][:, :, n0 + n4:n0 + n4 + 4]
                            P.copy('act', dstap, pG[gb][:], [f'pG{gb}'], [f'Gst{gs}'])

                    if parts:
                        parts[0]()
                    expand(0); onehot(0)
                    for pc in range(4):
                        if pc + 1 < 4:
                            expand(pc + 1); onehot(pc + 1)
                        gmm(pc)
                        if parts:
                            parts[pc + 1]()
                    P.dma('pool', Gs[ti], Gst[gs][:].rearrange("i j n -> i (j n)"), [f'Gst{gs}'], ['Gs'], f'Gst{gs}')

                if do_a:
                    s1big(0); s1(0)
                    for pf in s2_parts(0):
                        pf()
                    for ti in range(32):
                        if ti + 1 < 32:
                            if (ti + 1) % 4 == 0:
                                s1big((ti + 1) // 4)
                            s1(ti + 1)
                            nparts = s2_parts(ti + 1)
                        else:
                            nparts = None
                        s3(ti, nparts)
                P.bg = set()
                P.barrier(); P.flush()

            with ExitStack() as st:
                NSUB = 4; NB = 4
                fnw = SB(st, "fnw", [128, 8], F32)
                P.dma('sp', fnw[:], finw, (), ['fnw'], 'fnw')
                hnt = [SB(st, f"bhnt{i}", [128, 8, 256], BF16) for i in range(NSUB)]
                yacc = [SB(st, f"yacc{i}", [128, 8, 256], F32) for i in range(NSUB)]
                ub = [SB(st, f"ub{i}", [128, 4, 1024], BF16) for i in range(NB)]
                vb = [SB(st, f"vb{i}", [128, 4, 1024], BF16) for i in range(NB)]
                Gg = [SB(st, f"Gg{i}", [128, 8, 4, 128], BF16) for i in range(NB)]
                ge = [SB(st, f"ge{i}", [128, 256], F32) for i in range(2)]
                Hm = [SB(st, f"Hm{i}", [128, NSUB, 4, 256], BF16) for i in range(2)]
                zsq = SB(st, "zsq", [128, 8, 256], F32)
                rstd = SB(st, "erstd", [128, 256], F32)
                ot = SB(st, "ot", [128, 8, 256], F32)
                pY = [PS(st, f"pY{i}", [128, 2, 256]) for i in range(4)]
                pH = [PS(st, f"pH{i}", [128, 512]) for i in range(2)]
                pN = PS(st, "epN", [128, 512])
                li = 0; ci = 0; yi = 0
                for ps_ in range(4 if ('E' in phases or 'b' in phases) else 0):
                    tok0 = ps_ * 1024
                    for sub in range(NSUB):
                        p0 = NMETA + tok0 + sub * 256
                        P.dma('sp', hnt[sub][:], hnT_s[:, :, p0:p0 + 256].rearrange("k p t -> p k t"), ['hnT_s'], [f'bhnt{sub}'], f'bhnt{sub}')
                        P.dma('sp', yacc[sub][:], h1T_s[:, :, p0:p0 + 256].rearrange("k p t -> p k t"), ['h1T_s'], [f'yacc{sub}'], f'yacc{sub}')
                    for g in range(32):
                        c0 = 4 * g
                        li += 1; bs = li % NB; hset = li % 2
                        P.dma('sp', ub[bs][:], u2b[c0:c0 + 4].rearrange("c p f -> p c f"), (), [f'ub{bs}'], f'ub{bs}')
                        P.dma('act', vb[bs][:], v2b[c0:c0 + 4].rearrange("c p f -> p c f"), (), [f'vb{bs}'], f'vb{bs}')
                        P.dma('pool', Gg[bs][:].rearrange("i t c n -> i t (c n)"),
                              Gs[ps_ * 8:(ps_ + 1) * 8, :, c0 * 128:(c0 + 4) * 128].rearrange("t i f -> i t f"),
                              ['Gs'], [f'Gg{bs}'], f'Gg{bs}')
                        for sub in range(NSUB):
                            for cc in range(4):
                                ci += 1; hs_ = ci % 2
                                for k in range(8):
                                    P.mm(pH[hs_][:, :256], ub[bs][:, cc, k * 128:(k + 1) * 128], hnt[sub][:, k, :], k == 0, k == 7,
                                         [f'ub{bs}', f'bhnt{sub}'], [f'pH{hs_}'])
                                P.act(ge[hs_][:], pH[hs_][:, :256], AF.Gelu, [f'pH{hs_}'], [f'ge{hs_}'])
                                P.tt('dve', Hm[hset][:, sub, cc, :].rearrange("p (t n) -> p t n", t=2),
                                     ge[hs_][:].rearrange("p (t n) -> p t n", t=2), Gg[bs][:, 2 * sub:2 * sub + 2, cc, :], ALU.mult,
                                     [f'ge{hs_}', f'Gg{bs}'], [f'Hm{hset}_{sub}'])
                        for mh in range(2):
                            for sub in range(NSUB):
                                yi += 1; ys = yi % 2
                                for m in range(4):
                                    pyt = pY[ys * 2 + m // 2]
                                    for cc in range(4):
                                        P.mm(pyt[:, m % 2, :], vb[bs][:, cc, (mh * 4 + m) * 128:(mh * 4 + m + 1) * 128], Hm[hset][:, sub, cc, :],
                                             cc == 0, cc == 3, [f'vb{bs}', f'Hm{hset}_{sub}'], [f'pY{ys * 2 + m // 2}'])
                                for m2 in range(2):
                                    ya = yacc[sub][:, mh * 4 + m2 * 2:mh * 4 + m2 * 2 + 2, :]
                                    P.tt('dve', ya, pY[ys * 2 + m2][:], ya, ALU.add, [f'pY{ys * 2 + m2}', f'yacc{sub}'], [f'yacc{sub}'])
                    for sub in range(NSUB):
                        P.act(zsq[:], yacc[sub][:], AF.Square, [f'yacc{sub}'], ['zsq'])
                        for k in range(8):
                            P.mm(pN[:, :256], ones_f[:], zsq[:, k, :], k == 0, k == 7, ['ones_f', 'zsq'], ['epN'])
                        P.rsqrt_mean(rstd[:], pN[:, :256], 1024.0, ['epN'], ['erstd'])
                        for k in range(8):
                            P.stt('dve', ot[:, k, :], yacc[sub][:, k, :], fnw[:, k:k + 1], rstd[:], ALU.mult, ALU.mult, [f'yacc{sub}', 'fnw', 'erstd'], ['ot'])
                        t_o = tok0 + sub * 256
                        P.dma('pool', outT[:, :, t_o:t_o + 256].rearrange("k p t -> p k t"), ot[:], ['ot'], ['outT'], 'ot')
                P.barrier(); P.flush()
        P.barrier(); P.flush()
    return nc


def _consts():
    ident = np.eye(128, dtype=np.float32)
    s = np.arange(64)
    U = (s[:, None] <= s[None, :]).astype(np.float32)
    L = (s[:, None] > s[None, :]).astype(np.float32)
    k = np.arange(128)[:, None]; q = np.arange(512)[None, :]
    mask = np.concatenate([((j * 128 + k) <= q).astype(np.float32) for j in range(4)], axis=1)
    iota = np.broadcast_to(np.arange(128, dtype=np.float32)[None, :], (128, 128)).copy()
    return ident, U, L, mask, iota


def _rope_tables():
    inv_freq = (np.float32(10000.0) ** (-(np.arange(0, 64, 2, dtype=np.float32)) / np.float32(64))).astype(np.float32)
    ang = (np.arange(T, dtype=np.float32)[:, None] * inv_freq[None, :]).astype(np.float32)
    ang = np.concatenate([ang, ang], axis=-1)
    cos = np.cos(ang).astype(np.float32).T
    sin = np.sin(ang).astype(np.float32).T
    sgn = np.concatenate([-np.ones(32, np.float32), np.ones(32, np.float32)])[:, None]
    cosT = np.concatenate([cos, cos], axis=0)
    sinT = np.concatenate([sin * sgn, sin * sgn], axis=0)
    return np.ascontiguousarray(cosT), np.ascontiguousarray(sinT)


def make_in_maps(x, meta_tokens, mix_norm_w, w_in, rec_lb_logits, rec_norm_w, diff_lambda_q1, diff_lambda_k1,
                 diff_lambda_q2, diff_lambda_k2, diff_subln_w, w_out, ffn_norm_w, peer_w_query, peer_subkeys,
                 peer_u, peer_v, final_norm_w, cores=range(8)):
    f = lambda a: np.ascontiguousarray(np.asarray(a, dtype=np.float32))
    x = f(x); meta = f(meta_tokens)
    w = f(w_in)[0]
    idx = np.arange(512).reshape(4, 2, 64)
    pidx = np.roll(idx, -32, axis=2).reshape(-1)
    wq_, wk_ = w[:, 0:512], w[:, 512:1024]
    w_ext = np.concatenate([wq_, wq_[:, pidx], wk_, wk_[:, pidx], w[:, 1024:]], axis=1)
    w_ext = np.ascontiguousarray(w_ext.reshape(8, 128, 4608))
    vec8 = lambda v: np.ascontiguousarray(f(v).reshape(8, 128).T)
    lbl = f(rec_lb_logits)
    lbl_f = np.ascontiguousarray(lbl.reshape(2, 4, 128).transpose(2, 0, 1).reshape(128, 8))
    lbl_t = np.ascontiguousarray(lbl.reshape(1, 1024))
    recnw = np.ascontiguousarray(f(rec_norm_w)[0].T)
    sublnw = np.ascontiguousarray(f(diff_subln_w)[0].reshape(128, 1))
    lamp = np.ascontiguousarray(np.concatenate([f(diff_lambda_q1)[0], f(diff_lambda_k1)[0], f(diff_lambda_q2)[0],
                                                f(diff_lambda_k2)[0]]).reshape(1, 256))
    wo = np.ascontiguousarray(f(w_out)[0].reshape(8, 128, 1024))
    wq2 = np.ascontiguousarray(f(peer_w_query)[0].reshape(8, 128, 2048))
    sk = f(peer_subkeys)[0]
    skT = np.ascontiguousarray(sk.transpose(3, 0, 1, 2).reshape(128, 2048))
    u = f(peer_u)[0]; v = f(peer_v)[0]
    u2 = np.ascontiguousarray(u.reshape(128, 128, 8, 128).transpose(1, 3, 2, 0).reshape(128, 128, 1024))
    v2 = np.ascontiguousarray(v.reshape(128, 128, 1024).transpose(1, 0, 2))
    ident, U, L, mask, iota = _consts()
    cosT, sinT = _rope_tables()
    shared = dict(w_in=w_ext, mixw=vec8(mix_norm_w[0]), cosT=cosT, sinT=sinT, lbl_f=lbl_f, lbl_t=lbl_t, recnw=recnw,
                  sublnw=sublnw, lamp=lamp, w_out=wo, ffnw=vec8(ffn_norm_w[0]), finw=vec8(final_norm_w), wq=wq2, skT=skT,
                  u2=u2, v2=v2, c_ident=ident, c_U=U, c_L=L, c_mask=mask, c_iota=iota)
    maps = []
    for b in cores:
        h0 = np.concatenate([meta, x[b]], axis=0)
        hT0 = np.ascontiguousarray(h0.T.reshape(8, 128, T))
        m = dict(shared); m["hT0"] = hT0
        maps.append(m)
    return maps


_NC = None


def kernel(**inputs):
    global _NC
    if _NC is None:
        _NC = build()
    maps = make_in_maps(**inputs)
    res = run_bass_kernel_spmd(_NC, maps, core_ids=list(range(8)))
    out = np.empty((8, NT, 1024), dtype=np.float32)
    for b in range(8):
        out[b] = np.asarray(res.results[b]["outT"]).reshape(1024, NT).T
    return out
```

```python
import numpy as np
from contextlib import ExitStack
import concourse.bass as bass
import concourse.mybir as mybir
from concourse.bass_utils import run_bass_kernel_spmd

F32 = mybir.dt.float32
BF16 = mybir.dt.bfloat16
U32 = mybir.dt.uint32
ALU = mybir.AluOpType
AF = mybir.ActivationFunctionType
AX = mybir.AxisListType

T = 4112
NT = 4096
NMETA = 16
EPS = 1e-6
ENG = ['pe', 'act', 'dve', 'pool', 'sp']
SEM_LIMIT = 30000


class Tok:
    __slots__ = ('sem', 'val', 'eng', 'dma')

    def __init__(self, sem, val, eng, dma):
        self.sem = sem; self.val = val; self.eng = eng; self.dma = dma


class Prog:
    def __init__(self, nc, stack):
        self.nc = nc; self.stack = stack
        self.ops = {e: [] for e in ENG}
        self.esem = {}; self.ecnt = {}
        self.waited = {e: {} for e in ENG}
        self.lastw = {}; self.readers = {}
        self.dsem = {}
        self.nsem = 0
        self.allsems = {}
        self.rr = 0
        self.bg = set()

    def newsem(self):
        self.nsem += 1
        s = self.stack.enter_context(self.nc.semaphore(f"s{self.nsem}"))
        self.allsems[id(s)] = [s, 0]
        return s

    def _tok(self, eng, dma_key):
        if dma_key is None:
            if eng not in self.esem or self.ecnt[eng] >= SEM_LIMIT:
                self.esem[eng] = self.newsem(); self.ecnt[eng] = 0
            self.ecnt[eng] += 1
            t = Tok(self.esem[eng], self.ecnt[eng], eng, False)
        else:
            if dma_key not in self.dsem or self.dsem[dma_key][1] >= SEM_LIMIT:
                self.dsem[dma_key] = [self.newsem(), 0]
            self.dsem[dma_key][1] += 16
            t = Tok(self.dsem[dma_key][0], self.dsem[dma_key][1], eng, True)
        self.allsems[id(t.sem)][1] = t.val
        return t

    def add(self, eng, fn, reads=(), writes=(), dma_key=None):
        need = {}

        def want(t, kind):
            if (not t.dma) and t.eng == eng and dma_key is None and eng == 'pe':
                return
            k = id(t.sem)
            if k not in need or need[k][1] < t.val:
                need[k] = (t.sem, t.val)
        for k in reads:
            t = self.lastw.get(k)
            if t is not None:
                want(t, 'raw')
        for k in writes:
            t = self.lastw.get(k)
            if t is not None:
                want(t, 'waw')
            for r in self.readers.get(k, {}).values():
                want(r, 'war')
        waits = []
        wd = self.waited[eng]
        for k, (sem, val) in need.items():
            if wd.get(k, 0) >= val:
                continue
            wd[k] = val
            waits.append((sem, val))
        tok = self._tok(eng, dma_key)
        inc = 16 if dma_key is not None else 1

        def emit(e):
            for s, v in waits[1:]:
                e.wait_ge(s, v)
            inst = fn(e)
            if waits:
                inst._wait_ge(waits[0][0], waits[0][1])
            inst.then_inc(tok.sem, inc)
        self.ops[eng].append(emit)
        for k in reads:
            self.readers.setdefault(k, {})[id(tok.sem)] = tok
        for k in writes:
            self.lastw[k] = tok
            self.readers[k] = {}
        return tok

    def barrier(self):
        for e in ENG:
            wd = self.waited[e]
            for k, (s, v) in self.allsems.items():
                if k in self.bg:
                    continue
                if v > 0 and wd.get(k, 0) < v:
                    wd[k] = v
                    self.ops[e].append(lambda en, s=s, v=v: en.wait_ge(s, v))
        self.lastw = {}; self.readers = {}

    def flush(self):
        ops = self.ops
        with self.nc.Block() as block:
            @block.sync
            def _(e):
                for f in ops['sp']:
                    f(e)

            @block.tensor
            def _(e):
                for f in ops['pe']:
                    f(e)

            @block.vector
            def _(e):
                for f in ops['dve']:
                    f(e)

            @block.scalar
            def _(e):
                for f in ops['act']:
                    f(e)

            @block.gpsimd
            def _(e):
                for f in ops['pool']:
                    f(e)
        self.ops = {e: [] for e in ENG}

    def mm(self, out, lhsT, rhs, start, stop, r, w):
        return self.add('pe', lambda e: e.matmul(out, lhsT, rhs, start=start, stop=stop), r, w)

    def tr(self, out, in_, ident, r, w):
        return self.add('pe', lambda e: e.transpose(out, in_, ident), r, w)

    def act(self, out, in_, func, r, w, scale=1.0, bias=0.0):
        return self.add('act', lambda e: e.activation(out=out, in_=in_, func=func, bias=bias, scale=scale), r, w)

    def tt(self, eng, out, in0, in1, op, r, w):
        return self.add(eng, lambda e: e.tensor_tensor(out=out, in0=in0, in1=in1, op=op), r, w)

    def ts(self, eng, out, in0, s1, s2, op0, op1, r, w):
        if op1 is None:
            return self.add(eng, lambda e: e.tensor_scalar(out=out, in0=in0, scalar1=s1, scalar2=None, op0=op0), r, w)
        return self.add(eng, lambda e: e.tensor_scalar(out=out, in0=in0, scalar1=s1, scalar2=s2, op0=op0, op1=op1), r, w)

    def stt(self, eng, out, in0, scalar, in1, op0, op1, r, w):
        return self.add(eng, lambda e: e.scalar_tensor_tensor(out=out, in0=in0, scalar=scalar, in1=in1, op0=op0, op1=op1), r, w)

    def copy(self, eng, out, in_, r, w):
        if eng == 'act':
            return self.add('act', lambda e: e.copy(out=out, in_=in_), r, w)
        return self.add(eng, lambda e: e.tensor_copy(out=out, in_=in_), r, w)

    def dma(self, eng, out, in_, r, w, key):
        return self.add(eng, lambda e: e.dma_start(out=out, in_=in_), r, w, dma_key=key)

    def rsqrt_mean(self, out, in_, n, r, w, tmpkey=None):
        self.act(out, in_, AF.Sqrt, r, w, scale=1.0 / n, bias=EPS)
        self.add('dve', lambda e: e.reciprocal(out=out, in_=out), w, w)


def build(debug=False, phases="0ABCDE"):
    nc = bass.Bass("TRN2", target_bir_lowering=False)

    def din(name, shape, dt=F32):
        return nc.dram_tensor(name, list(shape), dt, kind="ExternalInput").ap()

    def dscr(name, shape, dt):
        if debug:
            return nc.dram_tensor(name, list(shape), dt, kind="ExternalOutput").ap()
        return nc.dram_tensor(name, list(shape), dt).ap()

    hT0 = din("hT0", [8, 128, T])
    w_in = din("w_in", [8, 128, 4608])
    mixw = din("mixw", [128, 8])
    cosT = din("cosT", [128, T])
    sinT = din("sinT", [128, T])
    lbl_f = din("lbl_f", [128, 8])
    lbl_t = din("lbl_t", [1, 1024])
    recnw = din("recnw", [128, 4])
    sublnw = din("sublnw", [128, 1])
    lamp = din("lamp", [1, 256])
    w_out = din("w_out", [8, 128, 1024])
    ffnw = din("ffnw", [128, 8])
    finw = din("finw", [128, 8])
    wq = din("wq", [8, 128, 2048])
    skT = din("skT", [128, 2048])
    u2 = din("u2", [128, 128, 1024])
    v2 = din("v2", [128, 128, 1024])
    c_ident = din("c_ident", [128, 128])
    c_U = din("c_U", [64, 64])
    c_L = din("c_L", [64, 64])
    c_mask = din("c_mask", [128, 2048])
    c_iota = din("c_iota", [128, 128])

    outT = nc.dram_tensor("outT", [8, 128, NT], F32, kind="ExternalOutput").ap()

    qT_s = dscr("qT_s", [4, 128, T], BF16)
    kT_s = dscr("kT_s", [4, 128, T], BF16)
    v_s = dscr("v_s", [T, 512], BF16)
    rqT_s = dscr("rqT_s", [4, 128, T], F32)
    rkT_s = dscr("rkT_s", [4, 128, T], F32)
    rgT_s = dscr("rgT_s", [4, 128, T], F32)
    rk_s = dscr("rk_s", [T, 512], F32)
    rg_s = dscr("rg_s", [T, 512], F32)
    ri_s = dscr("ri_s", [T, 512], BF16)
    mixT_s = dscr("mixT_s", [8, 128, T], BF16)
    h1T_s = dscr("h1T_s", [8, 128, T], F32)
    hnT_s = dscr("hnT_s", [8, 128, T], BF16)
    u2b = nc.dram_tensor("u2b", [128, 128, 1024], BF16).ap()
    v2b = nc.dram_tensor("v2b", [128, 128, 1024], BF16).ap()
    Gs = nc.dram_tensor("Gs", [32, 128, 128 * 128], BF16).ap()

    blocks = [(i * 512, 512) for i in range(8)] + [(4096, 16)]

    with ExitStack() as top:
        P = Prog(nc, top)

        def SB(st, name, shape, dt):
            return st.enter_context(nc.sbuf_tensor(name, list(shape), dt))

        def PS(st, name, shape, dt=F32):
            return st.enter_context(nc.psum_tensor(name, list(shape), dt))

        ones_f = SB(top, "ones_f", [128, 128], F32)
        ones_b = SB(top, "ones_b", [128, 128], BF16)
        ident = SB(top, "ident", [128, 128], F32)
        P.add('dve', lambda e: e.memset(ones_f[:], 1.0), (), ['ones_f'])
        P.add('dve', lambda e: e.memset(ones_b[:], 1.0), (), ['ones_b'])
        P.dma('sp', ident[:], c_ident, (), ['ident'], 'ident')

        if 'A' in phases:
            with ExitStack() as st:
                aT = SB(st, "aT", [128, 8, 1024], BF16)
                wb = SB(st, "wb", [128, 8, 4608], BF16)
                mw = SB(st, "mw", [128, 8], F32)
                lbf = SB(st, "lbf", [128, 8], F32)
                omlf = SB(st, "omlf", [128, 4], F32)
                lbt = SB(st, "lbt", [128, 1024], F32)
                omlt = SB(st, "omlt", [128, 512], F32)
                P.dma('sp', mw[:], mixw, (), ['mw'], 'mw')
                P.dma('sp', lbf[:], lbl_f, (), ['lbf'], 'lbf')
                P.dma('sp', lbt[:], lbl_t.partition_broadcast(128), (), ['lbt'], 'lbt')
                P.tt('dve', omlf[:], lbf[:, 4:8], lbf[:, 0:4], ALU.subtract, ['lbf'], ['omlf'])
                P.act(omlf[:], omlf[:], AF.Sigmoid, ['omlf'], ['omlf'])
                P.tt('dve', omlt[:], lbt[:, 512:1024], lbt[:, 0:512], ALU.subtract, ['lbt'], ['omlt'])
                P.act(omlt[:], omlt[:], AF.Sigmoid, ['omlt'], ['omlt'])
                for cg in range(9):
                    P.dma('pool', wb[:, :, cg * 512:(cg + 1) * 512], w_in[:, :, cg * 512:(cg + 1) * 512].rearrange("k p c -> p k c"),
                          (), ['wb'], 'wb')
                bg_list = []
                if '0' in phases:
                    for (src, dst, key) in ((u2, u2b, 'p0u'), (v2, v2b, 'p0v')):
                        for c0 in range(0, 128, 8):
                            bg_list.append((dst[c0:c0 + 8], src[c0:c0 + 8], key))

                def emit_bg(n):
                    for _ in range(n):
                        if bg_list:
                            d_, s_, k_ = bg_list.pop(0)
                            t = P.dma('pool', d_, s_, (), [], k_)
                            P.bg.add(id(t.sem))
                with ExitStack() as st2:
                    cs = [SB(st2, f"cs{i}", [128, 2, 512], F32) for i in range(2)]
                    NSTG = 6
                    stg = [SB(st2, f"stg{i}", [128, 512], F32) for i in range(NSTG)]
                    stgb = [SB(st2, f"stgb{i}", [128, 512], BF16) for i in range(NSTG)]
                    tmp = [SB(st2, f"tmp{i}", [128, 512], F32) for i in range(4)]
                    pp = [PS(st2, f"pp{i}", [128, 512]) for i in range(6)]
                    cnt = {'pp': 0, 'stg': 0, 'stgb': 0, 'tmp': 0}
                    xt = SB(st2, "xt0", [128, 8, 512], F32)
                    sq = SB(st2, "sq", [128, 8, 512], F32)
                    rstd = SB(st2, "rstd", [128, 512], F32)
                    ssp = PS(st2, "ssp", [128, 512])

                    def aoff(t):
                        return ((t // 512) % 2) * 512 + (t % 512)

                    def akey(t):
                        return f'aT{(t // 512) % 2}'

                    def norm(bi):
                        t0, n = blocks[bi]
                        P.dma('sp', xt[:, :, :n], hT0[:, :, t0:t0 + n].rearrange("k p t -> p k t"), (), ['xt0'], 'xt0')
                        P.act(sq[:, :, :n], xt[:, :, :n], AF.Square, ['xt0'], ['sq'])
                        for k in range(8):
                            P.mm(ssp[:, :n], ones_f[:], sq[:, k, :n], k == 0, k == 7, ['ones_f', 'sq'], ['ssp'])
                        P.rsqrt_mean(rstd[:, :n], ssp[:, :n], 1024.0, ['ssp'], ['rstd'])
                        o = aoff(t0)
                        for k in range(8):
                            P.stt('dve', aT[:, k, o:o + n], xt[:, k, :n], mw[:, k:k + 1], rstd[:, :n],
                                  ALU.mult, ALU.mult, ['xt0', 'mw', 'rstd'], [akey(t0)])

                    def nxt(kind, n):
                        cnt[kind] += 1
                        return cnt[kind] % n

                    def proj_f(pi, col0, t0, n):
                        for k in range(8):
                            P.mm(pp[pi][:, :n], wb[:, k, col0:col0 + 128], aT[:, k, aoff(t0):aoff(t0) + n], k == 0, k == 7,
                                 ['wb', akey(t0)], [f'pp{pi}'])

                    def proj_t(pi, col0, t0, r):
                        for k in range(8):
                            P.mm(pp[pi][:r, :], aT[:, k, aoff(t0):aoff(t0) + r], wb[:, k, col0:col0 + 512], k == 0, k == 7,
                                 ['wb', akey(t0)], [f'pp{pi}'])

                    norm(0)
                    for bi, (t0, n) in enumerate(blocks):
                        c = bi % 2
                        emit_bg(4)
                        P.dma('sp', cs[c][:, 0, :n], cosT[:, t0:t0 + n], (), [f'cs{c}'], f'cs{c}')
                        P.dma('sp', cs[c][:, 1, :n], sinT[:, t0:t0 + n], (), [f'cs{c}'], f'cs{c}')
                        for gi, (base, dst) in enumerate(((0, qT_s), (1024, kT_s))):
                            for h in range(4):
                                pa = nxt('pp', 6); proj_f(pa, base + h * 128, t0, n)
                                pb = nxt('pp', 6); proj_f(pb, base + 512 + h * 128, t0, n)
                                t1 = nxt('tmp', 4)
                                P.tt('dve', tmp[t1][:, :n], pp[pa][:, :n], cs[c][:, 0, :n], ALU.mult, [f'pp{pa}', f'cs{c}'], [f'tmp{t1}'])
                                t2 = nxt('tmp', 4)
                                P.tt('dve', tmp[t2][:, :n], pp[pb][:, :n], cs[c][:, 1, :n], ALU.mult, [f'pp{pb}', f'cs{c}'], [f'tmp{t2}'])
                                sb_ = nxt('stgb', NSTG)
                                P.tt('pool', stgb[sb_][:, :n], tmp[t1][:, :n], tmp[t2][:, :n], ALU.add, [f'tmp{t1}', f'tmp{t2}'], [f'stgb{sb_}'])
                                P.dma('pool', dst[h, :, t0:t0 + n], stgb[sb_][:, :n], [f'stgb{sb_}'], [], f'stgb{sb_}')
                        if bi + 1 < len(blocks):
                            norm(bi + 1)
                        for (base, dst) in ((2560, rqT_s), (4096, rgT_s)):
                            for h in range(4):
                                pa = nxt('pp', 6); proj_f(pa, base + h * 128, t0, n)
                                s_ = nxt('stg', NSTG)
                                P.act(stg[s_][:, :n], pp[pa][:, :n], AF.Silu, [f'pp{pa}'], [f'stg{s_}'])
                                P.dma('pool', dst[h, :, t0:t0 + n], stg[s_][:, :n], [f'stg{s_}'], [], f'stg{s_}')
                        for h in range(4):
                            pa = nxt('pp', 6); proj_f(pa, 3072 + h * 128, t0, n)
                            t1 = nxt('tmp', 4)
                            P.act(tmp[t1][:, :n], pp[pa][:, :n], AF.Sigmoid, [f'pp{pa}'], [f'tmp{t1}'], scale=-1.0)
                            s_ = nxt('stg', NSTG)
                            P.ts('pool', stg[s_][:, :n], tmp[t1][:, :n], omlf[:, h:h + 1], None, ALU.mult, None, [f'tmp{t1}', 'omlf'], [f'stg{s_}'])
                            P.dma('pool', rkT_s[h, :, t0:t0 + n], stg[s_][:, :n], [f'stg{s_}'], [], f'stg{s_}')
                        for r0 in range(0, n, 128):
                            r = min(128, n - r0)
                            tt0 = t0 + r0
                            for (base, dst) in ((2048, v_s), (3584, ri_s)):
                                pa = nxt('pp', 6); proj_t(pa, base, tt0, r)
                                sb_ = nxt('stgb', NSTG)
                                P.copy('act', stgb[sb_][:r, :], pp[pa][:r, :], [f'pp{pa}'], [f'stgb{sb_}'])
                                P.dma('pool', dst[tt0:tt0 + r, :], stgb[sb_][:r, :], [f'stgb{sb_}'], [], f'stgb{sb_}')
                            pa = nxt('pp', 6); proj_t(pa, 3072, tt0, r)
                            t1 = nxt('tmp', 4)
                            P.act(tmp[t1][:r, :], pp[pa][:r, :], AF.Sigmoid, [f'pp{pa}'], [f'tmp{t1}'], scale=-1.0)
                            s_ = nxt('stg', NSTG)
                            P.tt('pool', stg[s_][:r, :], tmp[t1][:r, :], omlt[:r, :], ALU.mult, [f'tmp{t1}', 'omlt'], [f'stg{s_}'])
                            P.dma('pool', rk_s[tt0:tt0 + r, :], stg[s_][:r, :], [f'stg{s_}'], [], f'stg{s_}')
                            s2 = nxt('stg', NSTG)
                            P.act(stg[s2][:r, :], stg[s_][:r, :], AF.Ln, [f'stg{s_}'], [f'stg{s2}'], scale=-1.0, bias=1.0)
                            P.dma('pool', rg_s[tt0:tt0 + r, :], stg[s2][:r, :], [f'stg{s2}'], [], f'stg{s2}')
                    emit_bg(64)
                    P.barrier(); P.flush()

        if 'B' in phases:
            with ExitStack() as st:
                kTh = [SB(st, f"kTh{i}", [64, 2, T], BF16) for i in range(2)]
                vh = [SB(st, f"vh{i}", [128, 33, 128], BF16) for i in range(2)]
                mskf = SB(st, "mskf", [128, 2048], F32)
                msk = SB(st, "msk", [128, 4, 512], BF16)
                qb_ = [SB(st, f"qb{i}", [64, 2, 512], BF16) for i in range(2)]
                pb_ = [SB(st, f"pb{i}", [128, 512], BF16) for i in range(8)]
                lam4 = SB(st, "lam4", [128, 256], F32)
                lamt = SB(st, "lamt", [128, 64], F32)
                lam2 = SB(st, "lam2", [128, 2], F32)
                neglam = SB(st, "neglam", [128, 1], F32)
                sw = SB(st, "sw", [128, 1], F32)
                rr = [SB(st, f"rr{i}", [128, 512], F32) for i in range(2)]
                to = [SB(st, f"to{i}", [128, 512], F32) for i in range(2)]
                oo = SB(st, "oo", [128, 512], F32)
                o2 = SB(st, "o2", [128, 512], F32)
                rs = SB(st, "rs", [128, 512], F32)
                ob = [SB(st, f"ob{i}", [128, 512], BF16) for i in range(2)]
                pS = [PS(st, f"pS{i}", [128, 512]) for i in range(4)]
                pO = [PS(st, f"pO{i}", [128, 512]) for i in range(2)]
                pL = [PS(st, f"pL{i}", [128, 512]) for i in range(2)]
                pN = pS[0]
                P.dma('sp', mskf[:], c_mask, (), ['mskf'], 'mskf')
                P.copy('dve', msk[:].rearrange("p a b -> p (a b)"), mskf[:], ['mskf'], ['msk'])
                P.dma('sp', lam4[:], lamp.partition_broadcast(128), (), ['lam4'], 'lam4')
                P.dma('sp', sw[:], sublnw, (), ['sw'], 'sw')
                P.ts('dve', sw[:], sw[:], 0.8, None, ALU.mult, None, ['sw'], ['sw'])
                for j in range(2):
                    P.tt('dve', lamt[:], lam4[:, j * 128:j * 128 + 64], lam4[:, j * 128 + 64:j * 128 + 128], ALU.mult, ['lam4'], ['lamt'])
                    P.add('dve', lambda e, j=j: e.reduce_sum(out=lam2[:, j:j + 1], in_=lamt[:], axis=AX.X), ['lamt'], ['lam2'])
                P.act(lam2[:], lam2[:], AF.Exp, ['lam2'], ['lam2'])
                P.tt('dve', neglam[:], lam2[:, 1:2], lam2[:, 0:1], ALU.subtract, ['lam2'], ['neglam'])
                P.ts('dve', neglam[:], neglam[:], -0.2, None, ALU.add, None, ['neglam'], ['neglam'])
                pbi = 0; qi = 0; oi = 0; pendB = []; pendF = []

                def fin_b(h, qs, nq, osl):
                    for comp in range(2):
                        P.add('dve', lambda e, comp=comp, nq=nq: e.reciprocal(out=rr[comp][:, :nq], in_=rr[comp][:, :nq]),
                              [f'rr{comp}'], [f'rr{comp}'])
                        P.tt('dve', to[comp][:, :nq], to[comp][:, :nq], rr[comp][:, :nq], ALU.mult, [f'to{comp}', f'rr{comp}'], [f'to{comp}'])
                    P.stt('dve', oo[:, :nq], to[1][:, :nq], neglam[:, 0:1], to[0][:, :nq], ALU.mult, ALU.add,
                          ['to0', 'to1', 'neglam'], ['oo'])
                    P.tt('dve', o2[:, :nq], oo[:, :nq], oo[:, :nq], ALU.mult, ['oo'], ['o2'])
                    P.mm(pN[:, :nq], ones_f[:], o2[:, :nq], True, True, ['ones_f', 'o2'], ['pS0'])
                    P.act(rs[:, :nq], pN[:, :nq], AF.Ln, ['pS0'], ['rs'], scale=1.0 / 128.0, bias=EPS)
                    P.act(rs[:, :nq], rs[:, :nq], AF.Exp, ['rs'], ['rs'], scale=-0.5)
                    P.stt('dve', ob[osl][:, :nq], oo[:, :nq], sw[:, 0:1], rs[:, :nq], ALU.mult, ALU.mult, ['oo', 'sw', 'rs'], [f'ob{osl}'])
                    P.dma('pool', mixT_s[h, :, qs:qs + nq], ob[osl][:, :nq], [f'ob{osl}'], ['mixT_s'], f'ob{osl}')

                def emit_ol(comp, kr, kt, psl, nq, nkt, hs):
                    P.mm(pO[comp][:, :nq], vh[hs][:kr, kt, :], pb_[psl][:kr, :nq], kt == 0, kt == nkt - 1,
                         [f'vh{hs}', f'pb{psl}'], [f'pO{comp}'])
                    P.mm(pL[comp][:, :nq], ones_b[:kr, :], pb_[psl][:kr, :nq], kt == 0, kt == nkt - 1,
                         ['ones_b', f'pb{psl}'], [f'pL{comp}'])

                for h in range(4):
                    hs = h % 2
                    P.dma('sp', kTh[hs][:], kT_s[h].rearrange("(c d) t -> d c t", c=2), ['kT_s'], [f'kTh{hs}'], f'kTh{hs}')
                    P.dma('sp', vh[hs][:, 0:32, :], v_s[0:4096, h * 128:(h + 1) * 128].rearrange("(tt p) d -> p tt d", p=128),
                          ['v_s'], [f'vh{hs}'], f'vh{hs}')
                    P.dma('sp', vh[hs][0:16, 32, :], v_s[4096:4112, h * 128:(h + 1) * 128], ['v_s'], [f'vh{hs}'], f'vh{hs}')
                    for (qs, nq) in blocks:
                        qi += 1; qsl = qi % 2
                        P.dma('sp', qb_[qsl][:, :, :nq], qT_s[h, :, qs:qs + nq].rearrange("(c d) t -> d c t", c=2),
                              ['qT_s'], [f'qb{qsl}'], f'qb{qsl}')
                        nkt = (qs + nq + 127) // 128
                        for kt in range(nkt):
                            kr = min(128, T - kt * 128)
                            for comp in range(2):
                                pbi += 1; sl = pbi % 4; psl = pbi % 8
                                P.mm(pS[sl][:kr, :nq], kTh[hs][:, comp, kt * 128:kt * 128 + kr], qb_[qsl][:, comp, :nq], True, True,
                                     [f'kTh{hs}', f'qb{qsl}'], [f'pS{sl}'])
                                P.act(pb_[psl][:kr, :nq], pS[sl][:kr, :nq], AF.Exp, [f'pS{sl}'], [f'pb{psl}'], scale=0.125)
                                if kt * 128 >= qs:
                                    j = (kt * 128 - qs) // 128
                                    P.tt('dve', pb_[psl][:kr, :nq], pb_[psl][:kr, :nq], msk[:kr, j, :nq], ALU.mult,
                                         [f'pb{psl}', 'msk'], [f'pb{psl}'])
                                pendB.append((comp, kr, kt, psl, nq, nkt, hs))
                            if len(pendB) > 2:
                                emit_ol(*pendB.pop(0)); emit_ol(*pendB.pop(0))
                            if pendF and (kt == min(3, nkt - 1)):
                                fin_b(*pendF.pop(0))
                        while pendB:
                            emit_ol(*pendB.pop(0))
                        for comp in range(2):
                            P.copy('dve', rr[comp][:, :nq], pL[comp][:, :nq], [f'pL{comp}'], [f'rr{comp}'])
                            P.copy('act', to[comp][:, :nq], pO[comp][:, :nq], [f'pO{comp}'], [f'to{comp}'])
                        oi += 1
                        pendF.append((h, qs, nq, oi % 2))
                while pendF:
                    fin_b(*pendF.pop(0))
                P.barrier(); P.flush()

        if 'C' in phases:
            with ExitStack() as st:
                U = SB(st, "U", [64, 64], F32)
                L = SB(st, "L", [64, 64], F32)
                nw = SB(st, "nw", [128, 4], F32)
                P.dma('sp', U[:], c_U, (), ['U'], 'U')
                P.dma('sp', L[:], c_L, (), ['L'], 'L')
                P.dma('sp', nw[:], recnw, (), ['nw'], 'nw')
                gtok = [SB(st, f"gtok{i}", [64, 8, 512], F32) for i in range(2)]
                ktok = [SB(st, f"ktok{i}", [64, 8, 512], F32) for i in range(2)]
                vtok = [SB(st, f"vtok{i}", [64, 8, 512], BF16) for i in range(2)]
                rqT = [SB(st, f"rqT{i}", [128, 4, 512], F32) for i in range(2)]
                rkT = [SB(st, f"rkT{i}", [128, 4, 512], F32) for i in range(2)]
                rgT = [SB(st, f"rgT{i}", [128, 4, 512], F32) for i in range(2)]
                E1 = SB(st, "E1", [128, 4, 512], F32)
                E2 = SB(st, "E2", [128, 512], F32)
                qe = SB(st, "qe", [128, 4, 512], BF16)
                ke = SB(st, "ke", [128, 4, 512], BF16)
                Ed = SB(st, "Ed", [64, 512], F32)
                kd = SB(st, "kd", [64, 8, 512], BF16)
                ATm = [SB(st, f"ATm{i}", [64, 64], BF16) for i in range(8)]
                S = [SB(st, f"S{i}", [128, 128], F32) for i in range(4)]
                Sb = [SB(st, f"Sb{i}", [128, 128], BF16) for i in range(4)]
                o2 = SB(st, "co2", [128, 512], F32)
                rs = SB(st, "crs", [128, 512], F32)
                rec = SB(st, "rec", [128, 512], F32)
                recb = [SB(st, f"recb{i}", [128, 512], BF16) for i in range(2)]
                pC = PS(st, "pC", [128, 512])
                pF = PS(st, "pF", [128, 512])
                pA = PS(st, "pA", [64, 8, 64])
                pOo = [PS(st, f"pOo{i}", [128, 512]) for i in range(4)]
                pSp = PS(st, "pSp", [128, 4, 128])
                for h in range(4):
                    P.add('dve', lambda e, h=h: e.memset(S[h][:], 0.0), (), [f'S{h}'])
                    P.add('pool', lambda e, h=h: e.memset(Sb[h][:], 0.0), (), [f'Sb{h}'])
                ri_ = 0
                for bi, (t0, n) in enumerate(blocks):
                    s = bi % 2
                    nch = (n + 63) // 64
                    cl = min(64, n)
                    P.dma('sp', gtok[s][:cl, :nch, :], rg_s[t0:t0 + n, :].rearrange("(ch p) c -> p ch c", p=cl), ['rg_s'], [f'gtok{s}'], f'gtok{s}')
                    P.dma('sp', ktok[s][:cl, :nch, :], rk_s[t0:t0 + n, :].rearrange("(ch p) c -> p ch c", p=cl), ['rk_s'], [f'ktok{s}'], f'ktok{s}')
                    P.dma('sp', vtok[s][:cl, :nch, :], ri_s[t0:t0 + n, :].rearrange("(ch p) c -> p ch c", p=cl), ['ri_s'], [f'vtok{s}'], f'vtok{s}')
                    P.dma('sp', rqT[s][:, :, :n], rqT_s[:, :, t0:t0 + n].rearrange("h p t -> p h t"), ['rqT_s'], [f'rqT{s}'], f'rqT{s}')
                    P.dma('sp', rkT[s][:, :, :n], rkT_s[:, :, t0:t0 + n].rearrange("h p t -> p h t"), ['rkT_s'], [f'rkT{s}'], f'rkT{s}')
                    P.dma('sp', rgT[s][:, :, :n], rgT_s[:, :, t0:t0 + n].rearrange("h p t -> p h t"), ['rgT_s'], [f'rgT{s}'], f'rgT{s}')
                    for h in range(4):
                        for ch in range(nch):
                            P.mm(pC[:, ch * 64:ch * 64 + cl], gtok[s][:cl, ch, h * 128:(h + 1) * 128], U[:cl, :cl], True, True,
                                 [f'gtok{s}', 'U'], ['pC'])
                        P.act(E1[:, h, :n], pC[:, :n], AF.Exp, ['pC'], [f'E1_{h}'])
                        P.act(E2[:, :n], pC[:, :n], AF.Exp, ['pC'], ['E2'], scale=-1.0)
                        P.tt('dve', qe[:, h, :n], rqT[s][:, h, :n], E1[:, h, :n], ALU.mult, [f'rqT{s}', f'E1_{h}'], [f'qe{h}'])
                        P.tt('pool', ke[:, h, :n], rkT[s][:, h, :n], E2[:, :n], ALU.mult, [f'rkT{s}', 'E2'], [f'ke{h}'])
                    for ch in range(nch):
                        P.mm(pF[:cl, :], L[:cl, :cl], gtok[s][:cl, ch, :], True, True, [f'gtok{s}', 'L'], ['pF'])
                        P.act(Ed[:cl, :], pF[:cl, :], AF.Exp, ['pF'], ['Ed'])
                        P.tt(('dve', 'pool')[ch % 2], kd[:cl, ch, :], ktok[s][:cl, ch, :], Ed[:cl, :], ALU.mult, [f'ktok{s}', 'Ed'], [f'kd{ch}'])
                    def stageA(ch):
                        c0 = ch * 64; st_ = ch % 2
                        for h in range(4):
                            P.mm(pA[:cl, st_ * 4 + h, :cl], ke[:, h, c0:c0 + cl], qe[:, h, c0:c0 + cl], True, True, [f'ke{h}', f'qe{h}'], ['pA'])

                    def stageA2(ch):
                        st_ = ch % 2
                        for h in range(4):
                            P.tt('dve', ATm[st_ * 4 + h][:cl, :cl], pA[:cl, st_ * 4 + h, :cl], U[:cl, :cl], ALU.mult, ['pA', 'U'], [f'ATm{st_}{h}'])

                    for ch in range(nch):
                        c0 = ch * 64; st_ = ch % 2
                        stageA(ch); stageA2(ch)
                        for h in range(4):
                            hc = slice(h * 128, (h + 1) * 128)
                            P.mm(pOo[h][:, c0:c0 + cl], Sb[h][:], qe[:, h, c0:c0 + cl], True, False, [f'Sb{h}', f'qe{h}'], [f'pOo{h}'])
                            P.mm(pOo[h][:, c0:c0 + cl], vtok[s][:cl, ch, hc], ATm[st_ * 4 + h][:cl, :cl], False, True, [f'vtok{s}', f'ATm{st_}{h}'], [f'pOo{h}'])
                            P.mm(pSp[:, h, :], kd[:cl, ch, hc], vtok[s][:cl, ch, hc], True, True, [f'kd{ch}', f'vtok{s}'], ['pSp'])
                        for h in range(4):
                            P.stt('dve', S[h][:], S[h][:], E1[:, h, c0 + cl - 1:c0 + cl], pSp[:, h, :], ALU.mult, ALU.add,
                                  [f'S{h}', f'E1_{h}', 'pSp'], [f'S{h}'])
                        for h in range(4):
                            P.copy('act', Sb[h][:], S[h][:], [f'S{h}'], [f'Sb{h}'])
                    for h in range(4):
                        P.act(o2[:, :n], pOo[h][:, :n], AF.Square, [f'pOo{h}'], ['co2'])
                        P.mm(pF[:, :n], ones_f[:], o2[:, :n], True, True, ['ones_f', 'co2'], ['pF'])
                        P.act(rs[:, :n], pF[:, :n], AF.Ln, ['pF'], ['crs'], scale=1.0 / 128.0, bias=EPS)
                        P.act(rs[:, :n], rs[:, :n], AF.Exp, ['crs'], ['crs'], scale=-0.5)
                        P.stt('dve', rec[:, :n], pOo[h][:, :n], nw[:, h:h + 1], rs[:, :n], ALU.mult, ALU.mult, [f'pOo{h}', 'nw', 'crs'], ['rec'])
                        ri_ += 1; rsl = ri_ % 2
                        P.tt('pool', recb[rsl][:, :n], rec[:, :n], rgT[s][:, h, :n], ALU.mult, ['rec', f'rgT{s}'], [f'recb{rsl}'])
                        P.dma('pool', mixT_s[4 + h, :, t0:t0 + n], recb[rsl][:, :n], [f'recb{rsl}'], ['mixT_s'], f'recb{rsl}')
                P.barrier(); P.flush()

        if 'D' in phases:
            with ExitStack() as st:
                wob = SB(st, "wob", [128, 8, 1024], BF16)
                fw = SB(st, "fw", [128, 8], F32)
                mixb = [SB(st, f"mixb{i}", [128, 8, 512], BF16) for i in range(2)]
                xt = [SB(st, f"dxt{i}", [128, 8, 512], F32) for i in range(2)]
                h1 = [SB(st, f"h1_{i}", [128, 8, 512], F32) for i in range(2)]
                sq = SB(st, "dsq", [128, 8, 512], F32)
                rstd = SB(st, "drstd", [128, 512], F32)
                hn = [SB(st, f"hn{i}", [128, 8, 512], BF16) for i in range(2)]
                pp = [PS(st, f"dpp{i}", [128, 512]) for i in range(4)]
                ssp = PS(st, "dssp", [128, 512])
                P.dma('sp', fw[:], ffnw, (), ['fw'], 'fw')
                for cg in range(2):
                    P.dma('pool', wob[:, :, cg * 512:(cg + 1) * 512], w_out[:, :, cg * 512:(cg + 1) * 512].rearrange("k p c -> p k c"),
                          (), ['wob'], 'wob')
                pi = [0]

                def d_proj(bi):
                    t0, n = blocks[bi]; s = bi % 2
                    P.dma('sp', mixb[s][:, :, :n], mixT_s[:, :, t0:t0 + n].rearrange("k p t -> p k t"), ['mixT_s'], [f'mixb{s}'], f'mixb{s}')
                    P.dma('sp', xt[s][:, :, :n], hT0[:, :, t0:t0 + n].rearrange("k p t -> p k t"), (), [f'dxt{s}'], f'dxt{s}')
                    for m in range(8):
                        pi[0] += 1; ps_ = pi[0] % 4
                        for k in range(8):
                            P.mm(pp[ps_][:, :n], wob[:, k, m * 128:(m + 1) * 128], mixb[s][:, k, :n], k == 0, k == 7,
                                 ['wob', f'mixb{s}'], [f'dpp{ps_}'])
                        P.tt('dve', h1[s][:, m, :n], pp[ps_][:, :n], xt[s][:, m, :n], ALU.add, [f'dpp{ps_}', f'dxt{s}'], [f'h1_{s}'])
                    P.dma('pool', h1T_s[:, :, t0:t0 + n].rearrange("k p t -> p k t"), h1[s][:, :, :n], [f'h1_{s}'], ['h1T_s'], f'h1_{s}')

                def d_post(bi):
                    t0, n = blocks[bi]; s = bi % 2
                    P.act(sq[:, :, :n], h1[s][:, :, :n], AF.Square, [f'h1_{s}'], ['dsq'])
                    for k in range(8):
                        P.mm(ssp[:, :n], ones_f[:], sq[:, k, :n], k == 0, k == 7, ['ones_f', 'dsq'], ['dssp'])
                    P.rsqrt_mean(rstd[:, :n], ssp[:, :n], 1024.0, ['dssp'], ['drstd'])
                    for k in range(8):
                        P.stt('dve', hn[s][:, k, :n], h1[s][:, k, :n], fw[:, k:k + 1], rstd[:, :n], ALU.mult, ALU.mult,
                              [f'h1_{s}', 'fw', 'drstd'], [f'hn{s}'])
                    P.dma('pool', hnT_s[:, :, t0:t0 + n].rearrange("k p t -> p k t"), hn[s][:, :, :n], [f'hn{s}'], ['hnT_s'], f'hn{s}')

                d_proj(0)
                for bi in range(len(blocks)):
                    if bi + 1 < len(blocks):
                        d_proj(bi + 1)
                    d_post(bi)
                P.barrier(); P.flush()

        if any(c in phases for c in 'Eab'):
            with ExitStack() as st:
                wqb = SB(st, "wqb", [128, 8, 2048], BF16)
                skb = SB(st, "skb", [128, 16, 128], BF16)
                iota = SB(st, "iota", [128, 128], F32)
                P.dma('sp', iota[:], c_iota, (), ['iota'], 'iota')
                for cg in range(4):
                    P.dma('pool', wqb[:, :, cg * 512:(cg + 1) * 512], wq[:, :, cg * 512:(cg + 1) * 512].rearrange("k p c -> p k c"),
                          (), ['wqb'], 'wqb')
                P.dma('pool', skb[:].rearrange("p a b -> p (a b)"), skT, (), ['skb'], 'skb')
                hnt4 = SB(st, "hnt4", [128, 8, 512], BF16)
                qpT4 = SB(st, "qpT4", [128, 16, 512], BF16)
                ssbs = [SB(st, f"ssb{i}", [128, 16, 128], F32) for i in range(2)]
                wk = SB(st, "wk", [128, 128], F32)
                sv = SB(st, "sv", [128, 16, 16], F32)
                si = SB(st, "si", [128, 16, 16], U32)
                sif = SB(st, "sif", [128, 16, 16], F32)
                cand = SB(st, "cand", [128, 8, 256], F32)
                wk2 = SB(st, "wk2", [128, 256], F32)
                tv = SB(st, "tv", [128, 8, 16], F32)
                pos = SB(st, "pos", [128, 8, 16], U32)
                au = SB(st, "au", [128, 8, 16], U32)
                bu = SB(st, "bu", [128, 8, 16], U32)
                af = SB(st, "af", [128, 8, 16], F32)
                bf = SB(st, "bf", [128, 8, 16], F32)
                ex = SB(st, "ex", [128, 8, 16], F32)
                zz = SB(st, "zz", [128, 8], F32)
                gate = SB(st, "gate", [128, 8, 16], F32)
                eq = SB(st, "eq", [128, 8, 16, 16], F32)
                eq2 = SB(st, "eq2", [128, 8, 16, 16], F32)
                idi = SB(st, "idi", [128, 8, 16], F32)
                idj = SB(st, "idj", [128, 8, 16], F32)
                tTs = [SB(st, f"tT{i}", [128, 3, 128], F32) for i in range(2)]
                OJ = [SB(st, f"OJ{i}", [128, 32, 128], BF16) for i in range(2)]
                OI = [SB(st, f"OI{i}", [128, 32, 128], BF16) for i in range(2)]
                iotab = SB(st, "iotab", [128, 128], BF16)
                iota3b = SB(st, "iota3b", [128, 32, 128], BF16)
                P.copy('dve', iotab[:], iota[:], ['iota'], ['iotab'])
                P.copy('dve', iota3b[:], iotab[:].unsqueeze(1).to_broadcast([128, 32, 128]), ['iotab'], ['iota3b'])
                Gst = [SB(st, f"Gst{i}", [128, 128, 128], BF16) for i in range(1)]
                pq = [PS(st, f"pq{i}", [128, 512]) for i in range(2)]
                psc = [PS(st, f"psc{i}", [128, 512]) for i in range(2)]
                pT = PS(st, "pT", [128, 3, 128])
                pG = [PS(st, f"pG{i}", [128, 128, 4]) for i in range(3)]
                gi = [0]
                do_a = ('E' in phases or 'a' in phases)

                def s1big(q4):
                    p0 = NMETA + q4 * 512
                    P.dma('sp', hnt4[:], hnT_s[:, :, p0:p0 + 512].rearrange("k p t -> p k t"), ['hnT_s'], ['hnt4'], 'hnt4')
                    for ch in range(16):
                        b_ = ch % 2
                        for k in range(8):
                            P.mm(pq[b_][:], wqb[:, k, ch * 128:(ch + 1) * 128], hnt4[:, k, :], k == 0, k == 7, ['wqb', 'hnt4'], [f'pq{b_}'])
                        P.copy('act', qpT4[:, ch, :], pq[b_][:], [f'pq{b_}'], ['qpT4'])

                def s1(ti):
                    s = ti % 2
                    off = (ti % 4) * 128
                    for g4 in range(4):
                        b_ = g4 % 2
                        for c4 in range(4):
                            ch = g4 * 4 + c4
                            P.mm(psc[b_][:, c4 * 128:(c4 + 1) * 128], qpT4[:, ch, off:off + 128], skb[:, ch, :], True, True, ['qpT4', 'skb'], [f'psc{b_}'])
                        P.copy('act', ssbs[s][:, g4 * 4:(g4 + 1) * 4, :].rearrange("p a b -> p (a b)"), psc[b_][:], [f'psc{b_}'], [f'ssb{s}'])

                def s2_parts(ti):
                    s = ti % 2
                    ssb = ssbs[s]; sk = f'ssb{s}'
                    sv4 = sv[:].rearrange("p (h two) k -> p h two k", two=2)
                    sif4 = sif[:].rearrange("p (h two) k -> p h two k", two=2)

                    def lvl1(g0, g1):
                        for g in range(g0, g1):
                            P.add('dve', lambda e, g=g: e.max(out=sv[:, g, 0:8], in_=ssb[:, g, :]), [sk], ['sv'])
                            P.add('dve', lambda e, g=g: e.max_index(out=si[:, g, 0:8], in_max=sv[:, g, 0:8], in_values=ssb[:, g, :]), [sk, 'sv'], ['si'])
                            P.add('dve', lambda e, g=g: e.match_replace(out=wk[:], in_to_replace=sv[:, g, 0:8], in_values=ssb[:, g, :], imm_value=-1e30),
                                  [sk, 'sv'], ['wk'])
                            P.add('dve', lambda e, g=g: e.max(out=sv[:, g, 8:16], in_=wk[:]), ['wk'], ['sv'])
                            P.add('dve', lambda e, g=g: e.max_index(out=si[:, g, 8:16], in_max=sv[:, g, 8:16], in_values=wk[:]), ['wk', 'sv'], ['si'])

                    def pre():
                        lvl1(0, 4)

                    def part0():
                        lvl1(4, 8)

                    def part1():
                        lvl1(8, 16)
                        P.copy('pool', sif[:], si[:], ['si'], ['sif'])
                        P.tt('dve', cand[:].rearrange("p h (a b) -> p h a b", a=16),
                             sv4[:, :, 0, :].unsqueeze(3).to_broadcast([128, 8, 16, 16]),
                             sv4[:, :, 1, :].unsqueeze(2).to_broadcast([128, 8, 16, 16]), ALU.add, ['sv'], ['cand'])

                    def part2():
                        for h in range(8):
                            P.add('dve', lambda e, h=h: e.max(out=tv[:, h, 0:8], in_=cand[:, h, :]), ['cand'], ['tv'])
                            P.add('dve', lambda e, h=h: e.max_index(out=pos[:, h, 0:8], in_max=tv[:, h, 0:8], in_values=cand[:, h, :]), ['cand', 'tv'], ['pos'])
                            P.add('dve', lambda e, h=h: e.match_replace(out=wk2[:], in_to_replace=tv[:, h, 0:8], in_values=cand[:, h, :], imm_value=-1e30),
                                  ['cand', 'tv'], ['wk2'])
                            P.add('dve', lambda e, h=h: e.max(out=tv[:, h, 8:16], in_=wk2[:]), ['wk2'], ['tv'])
                            P.add('dve', lambda e, h=h: e.max_index(out=pos[:, h, 8:16], in_max=tv[:, h, 8:16], in_values=wk2[:]), ['wk2', 'tv'], ['pos'])

                    def part3():
                        P.tt('pool', ex[:], tv[:], tv[:, :, 0:1].to_broadcast([128, 8, 16]), ALU.subtract, ['tv'], ['ex'])
                        P.act(ex[:], ex[:], AF.Exp, ['ex'], ['ex'])
                        P.add('dve', lambda e: e.reduce_sum(out=zz[:], in_=ex[:], axis=AX.X), ['ex'], ['zz'])
                        P.add('dve', lambda e: e.reciprocal(out=zz[:], in_=zz[:]), ['zz'], ['zz'])
                        P.tt('pool', gate[:], ex[:], zz[:].unsqueeze(2).to_broadcast([128, 8, 16]), ALU.mult, ['ex', 'zz'], ['gate'])
                        P.add('dve', lambda e: e.tensor_single_scalar(out=au[:], in_=pos[:], scalar=4, op=ALU.logical_shift_right), ['pos'], ['au'])
                        P.add('dve', lambda e: e.tensor_single_scalar(out=bu[:], in_=pos[:], scalar=15, op=ALU.bitwise_and), ['pos'], ['bu'])
                        P.copy('pool', af[:], au[:], ['au'], ['af'])
                        P.copy('pool', bf[:], bu[:], ['bu'], ['bf'])
                        io16 = iota[:, 0:16].unsqueeze(1).unsqueeze(1).to_broadcast([128, 8, 16, 16])
                        for (src, half, dst, nm, eqt, ek) in ((af, 0, idi, 'idi', eq, 'eq'), (bf, 1, idj, 'idj', eq2, 'eq2')):
                            P.tt('dve', eqt[:], src[:].unsqueeze(3).to_broadcast([128, 8, 16, 16]), io16, ALU.is_equal, [('af', 'bf')[half], 'iota'], [ek])
                            P.tt('dve', eqt[:], eqt[:], sif4[:, :, half, :].unsqueeze(2).to_broadcast([128, 8, 16, 16]), ALU.mult, [ek, 'sif'], [ek])
                        for (dst, nm, eqt, ek) in ((idi, 'idi', eq, 'eq'), (idj, 'idj', eq2, 'eq2')):
                            P.add('dve', lambda e, dst=dst, eqt=eqt: e.reduce_sum(out=dst[:], in_=eqt[:], axis=AX.X), [ek], [nm])
                        for j, (src, nm) in enumerate(((idi, 'idi'), (idj, 'idj'), (gate, 'gate'))):
                            P.tr(pT[:, j, :], src[:].rearrange("p h k -> p (h k)"), ident[:], [nm, 'ident'], ['pT'])
                        P.copy('act', tTs[s][:], pT[:], ['pT'], [f'tT{s}'])

                    return [pre, part0, part1, part2, part3]

                def s3(ti, parts):
                    s = ti % 2
                    tT = tTs[s]; tk = f'tT{s}'
                    gs = 0

                    def expand(pc):
                        half = pc % 2; n0 = pc * 32
                        P.act(OJ[half][:], tT[:, 1, n0:n0 + 32].unsqueeze(2).to_broadcast([128, 32, 128]), AF.Copy, [tk], [f'OJ{half}'])
                        P.act(OI[half][:], tT[:, 0, n0:n0 + 32].unsqueeze(2).to_broadcast([128, 32, 128]), AF.Copy, [tk], [f'OI{half}'])

                    def onehot(pc):
                        half = pc % 2; n0 = pc * 32
                        P.tt('dve', OJ[half][:], OJ[half][:], iota3b[:], ALU.is_equal, [f'OJ{half}', 'iota3b'], [f'OJ{half}'])
                        P.tt('dve', OI[half][:], OI[half][:], iota3b[:], ALU.is_equal, [f'OI{half}', 'iota3b'], [f'OI{half}'])
                        P.tt('pool', OI[half][:], OI[half][:], tT[:, 2, n0:n0 + 32].unsqueeze(2).to_broadcast([128, 32, 128]), ALU.mult,
                             [f'OI{half}', tk], [f'OI{half}'])

                    def gmm(pc):
                        half = pc % 2; n0 = pc * 32
                        for n4 in range(0, 32, 4):
                            gi[0] += 1; gb = gi[0] % 3
                            for q in range(4):
                                nn = n4 + q
                                P.mm(pG[gb][:, :, q], OI[half][:, nn, :], OJ[half][:, nn, :], True, True, [f'OI{half}', f'OJ{half}'], [f'pG{gb}'])
                            dstap = Gst[gs][:, :, n0 + n4:n0 + n4 + 4]
                            P.copy('act', dstap, pG[gb][:], [f'pG{gb}'], [f'Gst{gs}'])

                    if parts:
                        parts[0]()
                    expand(0); onehot(0)
                    for pc in range(4):
                        if pc + 1 < 4:
                            expand(pc + 1); onehot(pc + 1)
                        gmm(pc)
                        if parts:
                            parts[pc + 1]()
                    P.dma('pool', Gs[ti], Gst[gs][:].rearrange("i j n -> i (j n)"), [f'Gst{gs}'], ['Gs'], f'Gst{gs}')

                if do_a:
                    s1big(0); s1(0)
                    for pf in s2_parts(0):
                        pf()
                    for ti in range(32):
                        if ti + 1 < 32:
                            if (ti + 1) % 4 == 0:
                                s1big((ti + 1) // 4)
                            s1(ti + 1)
                            nparts = s2_parts(ti + 1)
                        else:
                            nparts = None
                        s3(ti, nparts)
                P.bg = set()
                P.barrier(); P.flush()

            with ExitStack() as st:
                NSUB = 4; NB = 3
                fnw = SB(st, "fnw", [128, 8], F32)
                P.dma('sp', fnw[:], finw, (), ['fnw'], 'fnw')
                hnt = [SB(st, f"bhnt{i}", [128, 8, 256], BF16) for i in range(NSUB)]
                yacc = [SB(st, f"yacc{i}", [128, 8, 256], F32) for i in range(NSUB)]
                ub = [SB(st, f"ub{i}", [128, 4, 1024], BF16) for i in range(NB)]
                vb = [SB(st, f"vb{i}", [128, 4, 1024], BF16) for i in range(NB)]
                Gg = [SB(st, f"Gg{i}", [128, 8, 4, 128], BF16) for i in range(NB)]
                ge = [SB(st, f"ge{i}", [128, 256], F32) for i in range(2)]
                Hm = [SB(st, f"Hm{i}", [128, NSUB, 4, 256], BF16) for i in range(2)]
                zsq = SB(st, "zsq", [128, 8, 256], F32)
                rstd = SB(st, "erstd", [128, 256], F32)
                ot = SB(st, "ot", [128, 8, 256], F32)
                pY = [PS(st, f"pY{i}", [128, 2, 256]) for i in range(4)]
                pH = [PS(st, f"pH{i}", [128, 512]) for i in range(2)]
                pN = PS(st, "epN", [128, 512])
                li = 0; ci = 0; yi = 0
                for ps_ in range(4 if ('E' in phases or 'b' in phases) else 0):
                    tok0 = ps_ * 1024
                    for sub in range(NSUB):
                        p0 = NMETA + tok0 + sub * 256
                        P.dma('sp', hnt[sub][:], hnT_s[:, :, p0:p0 + 256].rearrange("k p t -> p k t"), ['hnT_s'], [f'bhnt{sub}'], f'bhnt{sub}')
                        P.dma('sp', yacc[sub][:], h1T_s[:, :, p0:p0 + 256].rearrange("k p t -> p k t"), ['h1T_s'], [f'yacc{sub}'], f'yacc{sub}')
                    for g in range(32):
                        c0 = 4 * g
                        li += 1; bs = li % NB; hset = li % 2
                        P.dma('sp', ub[bs][:], u2b[c0:c0 + 4].rearrange("c p f -> p c f"), (), [f'ub{bs}'], f'ub{bs}')
                        P.dma('act', vb[bs][:], v2b[c0:c0 + 4].rearrange("c p f -> p c f"), (), [f'vb{bs}'], f'vb{bs}')
                        P.dma('pool', Gg[bs][:].rearrange("i t c n -> i t (c n)"),
                              Gs[ps_ * 8:(ps_ + 1) * 8, :, c0 * 128:(c0 + 4) * 128].rearrange("t i f -> i t f"),
                              ['Gs'], [f'Gg{bs}'], f'Gg{bs}')
                        for sub in range(NSUB):
                            for cc in range(4):
                                ci += 1; hs_ = ci % 2
                                for k in range(8):
                                    P.mm(pH[hs_][:, :256], ub[bs][:, cc, k * 128:(k + 1) * 128], hnt[sub][:, k, :], k == 0, k == 7,
                                         [f'ub{bs}', f'bhnt{sub}'], [f'pH{hs_}'])
                                P.act(ge[hs_][:], pH[hs_][:, :256], AF.Gelu, [f'pH{hs_}'], [f'ge{hs_}'])
                                P.tt('dve', Hm[hset][:, sub, cc, :].rearrange("p (t n) -> p t n", t=2),
                                     ge[hs_][:].rearrange("p (t n) -> p t n", t=2), Gg[bs][:, 2 * sub:2 * sub + 2, cc, :], ALU.mult,
                                     [f'ge{hs_}', f'Gg{bs}'], [f'Hm{hset}_{sub}'])
                        for mh in range(2):
                            for sub in range(NSUB):
                                yi += 1; ys = yi % 2
                                for m in range(4):
                                    pyt = pY[ys * 2 + m // 2]
                                    for cc in range(4):
                                        P.mm(pyt[:, m % 2, :], vb[bs][:, cc, (mh * 4 + m) * 128:(mh * 4 + m + 1) * 128], Hm[hset][:, sub, cc, :],
                                             cc == 0, cc == 3, [f'vb{bs}', f'Hm{hset}_{sub}'], [f'pY{ys * 2 + m // 2}'])
                                for m2 in range(2):
                                    ya = yacc[sub][:, mh * 4 + m2 * 2:mh * 4 + m2 * 2 + 2, :]
                                    P.tt('dve', ya, pY[ys * 2 + m2][:], ya, ALU.add, [f'pY{ys * 2 + m2}', f'yacc{sub}'], [f'yacc{sub}'])
                    for sub in range(NSUB):
                        P.act(zsq[:], yacc[sub][:], AF.Square, [f'yacc{sub}'], ['zsq'])
                        for k in range(8):
                            P.mm(pN[:, :256], ones_f[:], zsq[:, k, :], k == 0, k == 7, ['ones_f', 'zsq'], ['epN'])
                        P.rsqrt_mean(rstd[:], pN[:, :256], 1024.0, ['epN'], ['erstd'])
                        for k in range(8):
                            P.stt('dve', ot[:, k, :], yacc[sub][:, k, :], fnw[:, k:k + 1], rstd[:], ALU.mult, ALU.mult, [f'yacc{sub}', 'fnw', 'erstd'], ['ot'])
                        t_o = tok0 + sub * 256
                        P.dma('pool', outT[:, :, t_o:t_o + 256].rearrange("k p t -> p k t"), ot[:], ['ot'], ['outT'], 'ot')
                P.barrier(); P.flush()
        P.barrier(); P.flush()
    return nc


def _consts():
    ident = np.eye(128, dtype=np.float32)
    s = np.arange(64)
    U = (s[:, None] <= s[None, :]).astype(np.float32)
    L = (s[:, None] > s[None, :]).astype(np.float32)
    k = np.arange(128)[:, None]; q = np.arange(512)[None, :]
    mask = np.concatenate([((j * 128 + k) <= q).astype(np.float32) for j in range(4)], axis=1)
    iota = np.broadcast_to(np.arange(128, dtype=np.float32)[None, :], (128, 128)).copy()
    return ident, U, L, mask, iota


def _rope_tables():
    inv_freq = (np.float32(10000.0) ** (-(np.arange(0, 64, 2, dtype=np.float32)) / np.float32(64))).astype(np.float32)
    ang = (np.arange(T, dtype=np.float32)[:, None] * inv_freq[None, :]).astype(np.float32)
    ang = np.concatenate([ang, ang], axis=-1)
    cos = np.cos(ang).astype(np.float32).T
    sin = np.sin(ang).astype(np.float32).T
    sgn = np.concatenate([-np.ones(32, np.float32), np.ones(32, np.float32)])[:, None]
    cosT = np.concatenate([cos, cos], axis=0)
    sinT = np.concatenate([sin * sgn, sin * sgn], axis=0)
    return np.ascontiguousarray(cosT), np.ascontiguousarray(sinT)


def make_in_maps(x, meta_tokens, mix_norm_w, w_in, rec_lb_logits, rec_norm_w, diff_lambda_q1, diff_lambda_k1,
                 diff_lambda_q2, diff_lambda_k2, diff_subln_w, w_out, ffn_norm_w, peer_w_query, peer_subkeys,
                 peer_u, peer_v, final_norm_w, cores=range(8)):
    f = lambda a: np.ascontiguousarray(np.asarray(a, dtype=np.float32))
    x = f(x); meta = f(meta_tokens)
    w = f(w_in)[0]
    idx = np.arange(512).reshape(4, 2, 64)
    pidx = np.roll(idx, -32, axis=2).reshape(-1)
    wq_, wk_ = w[:, 0:512], w[:, 512:1024]
    w_ext = np.concatenate([wq_, wq_[:, pidx], wk_, wk_[:, pidx], w[:, 1024:]], axis=1)
    w_ext = np.ascontiguousarray(w_ext.reshape(8, 128, 4608))
    vec8 = lambda v: np.ascontiguousarray(f(v).reshape(8, 128).T)
    lbl = f(rec_lb_logits)
    lbl_f = np.ascontiguousarray(lbl.reshape(2, 4, 128).transpose(2, 0, 1).reshape(128, 8))
    lbl_t = np.ascontiguousarray(lbl.reshape(1, 1024))
    recnw = np.ascontiguousarray(f(rec_norm_w)[0].T)
    sublnw = np.ascontiguousarray(f(diff_subln_w)[0].reshape(128, 1))
    lamp = np.ascontiguousarray(np.concatenate([f(diff_lambda_q1)[0], f(diff_lambda_k1)[0], f(diff_lambda_q2)[0],
                                                f(diff_lambda_k2)[0]]).reshape(1, 256))
    wo = np.ascontiguousarray(f(w_out)[0].reshape(8, 128, 1024))
    wq2 = np.ascontiguousarray(f(peer_w_query)[0].reshape(8, 128, 2048))
    sk = f(peer_subkeys)[0]
    skT = np.ascontiguousarray(sk.transpose(3, 0, 1, 2).reshape(128, 2048))
    u = f(peer_u)[0]; v = f(peer_v)[0]
    u2 = np.ascontiguousarray(u.reshape(128, 128, 8, 128).transpose(1, 3, 2, 0).reshape(128, 128, 1024))
    v2 = np.ascontiguousarray(v.reshape(128, 128, 1024).transpose(1, 0, 2))
    ident, U, L, mask, iota = _consts()
    cosT, sinT = _rope_tables()
    shared = dict(w_in=w_ext, mixw=vec8(mix_norm_w[0]), cosT=cosT, sinT=sinT, lbl_f=lbl_f, lbl_t=lbl_t, recnw=recnw,
                  sublnw=sublnw, lamp=lamp, w_out=wo, ffnw=vec8(ffn_norm_w[0]), finw=vec8(final_norm_w), wq=wq2, skT=skT,
                  u2=u2, v2=v2, c_ident=ident, c_U=U, c_L=L, c_mask=mask, c_iota=iota)
    maps = []
    for b in cores:
        h0 = np.concatenate([meta, x[b]], axis=0)
        hT0 = np.ascontiguousarray(h0.T.reshape(8, 128, T))
        m = dict(shared); m["hT0"] = hT0
        maps.append(m)
    return maps


_NC = None


def kernel(**inputs):
    global _NC
    if _NC is None:
        _NC = build()
    maps = make_in_maps(**inputs)
    res = run_bass_kernel_spmd(_NC, maps, core_ids=list(range(8)))
    out = np.empty((8, NT, 1024), dtype=np.float32)
    for b in range(8):
        out[b] = np.asarray(res.results[b]["outT"]).reshape(1024, NT).T
    return out
```
